# Optimizing a Trainium2 kernel written in Bass

```python
import jax
import jax.numpy as jnp
from jax import lax
import numpy as np

D_MODEL = 2048
BATCH = 16
SEQ = 2048
DEPTH = 2

CONV_WIDTH = 1024
CONV_KERNEL = 31
RWKV_HEAD_SIZE = 64
RWKV_HEADS = 32
RWKV_WIDTH = RWKV_HEADS * RWKV_HEAD_SIZE
R_DECAY = 96
R_AAA = 96
R_GATE = 256
R_MV = 64
RWKV_COLS = 3 * RWKV_WIDTH + R_DECAY + R_AAA + R_GATE
IN_COLS = 2 * CONV_WIDTH + RWKV_COLS + 2 * D_MODEL
N_EXPERTS = 32
TOP_K = 4
D_EXPERT = 1024
SWIGLU_LIMIT = 7.0
SWIGLU_ALPHA = 1.702
EXPERT_BLOCK = 128

RMS_EPS = 1e-5
LN_EPS = 1e-5
GN_EPS = 64e-5

kernel_name = 'cond_hybrid_conv_rwkv7_moe'


def rmsnorm(x, g):
    xf = x.astype(jnp.float32)
    xf = xf * lax.rsqrt(jnp.mean(xf * xf, axis=-1, keepdims=True) + RMS_EPS)
    return xf.astype(x.dtype) * g


def layernorm(x, g, b):
    xf = x.astype(jnp.float32)
    mu = jnp.mean(xf, axis=-1, keepdims=True)
    var = jnp.mean(jnp.square(xf - mu), axis=-1, keepdims=True)
    return ((xf - mu) * lax.rsqrt(var + LN_EPS)).astype(x.dtype) * g + b


def token_shift(z, mu):
    prev = jnp.pad(z[:, :-1], ((0, 0), (1, 0), (0, 0)))
    return z + (prev - z) * mu


def conformer_conv_branch(glu_in, conv_w, conv_b, ln_g, ln_b, w_proj):
    a = glu_in[..., :CONV_WIDTH] * jax.nn.sigmoid(glu_in[..., CONV_WIDTH:])
    a = lax.conv_general_dilated(
        a, conv_w.astype(a.dtype), window_strides=(1,),
        padding=[(CONV_KERNEL - 1, 0)],
        dimension_numbers=('NWC', 'WIO', 'NWC'),
        feature_group_count=CONV_WIDTH) + conv_b
    a = jax.nn.silu(layernorm(a, ln_g, ln_b))
    return a @ w_proj


def wkv7_scan(r, decay, k, v, a, b):
    bsz, _, h, n = r.shape
    xs = tuple(jnp.moveaxis(t, 1, 0) for t in (r, decay, k, v, a, b))

    def step(state, inp):
        r_t, d_t, k_t, v_t, a_t, b_t = inp
        sa = jnp.einsum('bhij,bhj->bhi', state, a_t)
        state = (state * d_t[:, :, None, :]
                 + v_t[..., :, None] * k_t[..., None, :]
                 + sa[..., :, None] * b_t[..., None, :])
        return state, jnp.einsum('bhij,bhj->bhi', state, r_t)

    state0 = jnp.zeros((bsz, h, n, n), jnp.float32)
    _, ys = lax.scan(step, state0, xs)
    return jnp.moveaxis(ys, 0, 1)


def rwkv7_branch(z, u, w0, w2, a0, a2, g2, k_k, k_a, r_k, gn_g, gn_b, w_proj, v_first, vres):
    bsz, seq, _ = z.shape
    cw = RWKV_WIDTH
    r = z[..., :cw]
    k = z[..., cw:2 * cw]
    v = z[..., 2 * cw:3 * cw]
    o = 3 * cw
    zw = z[..., o:o + R_DECAY]
    o += R_DECAY
    za = z[..., o:o + R_AAA]
    o += R_AAA
    zg = z[..., o:o + R_GATE]
    w = -jax.nn.softplus(-(w0 + jnp.tanh(zw) @ w2)) - 0.5
    a = jax.nn.sigmoid(a0 + za @ a2)
    g = jax.nn.sigmoid(zg) @ g2
    if vres is None:
        v_first = v
    else:
        v0, v1, mu_v, v2 = vres
        zv = token_shift(u @ v1, mu_v)
        v = v + (v_first - v) * jax.nn.sigmoid(v0 + zv @ v2)
    kk = k * k_k
    k = k * (1 + (a - 1) * k_a)

    def heads(t):
        return t.astype(jnp.float32).reshape(bsz, seq, RWKV_HEADS, RWKV_HEAD_SIZE)

    rh, kh, vh, ah, wh, kkh = heads(r), heads(k), heads(v), heads(a), heads(w), heads(kk)
    kkh = kkh / jnp.maximum(jnp.linalg.norm(kkh, axis=-1, keepdims=True), 1e-12)
    y = wkv7_scan(rh, jnp.exp(-jnp.exp(wh)), kh, vh, -kkh, kkh * ah)
    mu = jnp.mean(y, axis=-1, keepdims=True)
    var = jnp.mean(jnp.square(y - mu), axis=-1, keepdims=True)
    hshape = (RWKV_HEADS, RWKV_HEAD_SIZE)
    y = ((y - mu) * lax.rsqrt(var + GN_EPS) * gn_g.reshape(hshape).astype(jnp.float32)
         + gn_b.reshape(hshape).astype(jnp.float32))
    y = y + jnp.sum(rh * kh * r_k.reshape(hshape).astype(jnp.float32), axis=-1, keepdims=True) * vh
    y = y.reshape(bsz, seq, cw).astype(u.dtype)
    return (y * g) @ w_proj, v_first


def moe_ffn(h, router_w, router_b, w_gate_up, b_gate_up, w_down, b_down):
    bsz, seq, d = h.shape
    t = bsz * seq
    hf = h.reshape(t, d)
    logits = (hf @ router_w + router_b).astype(jnp.float32)
    top_logits, top_idx = lax.top_k(logits, TOP_K)
    top_w = jax.nn.softmax(top_logits, axis=-1).astype(h.dtype)
    m = t * TOP_K
    flat_e = top_idx.reshape(m)
    flat_tok = jnp.arange(m, dtype=jnp.int32) // TOP_K
    order = jnp.argsort(flat_e)
    sorted_e = flat_e[order]
    counts = jnp.bincount(flat_e, length=N_EXPERTS)
    padded = (counts + EXPERT_BLOCK - 1) // EXPERT_BLOCK * EXPERT_BLOCK
    starts = jnp.cumsum(counts) - counts
    pad_ends = jnp.cumsum(padded)
    pad_starts = pad_ends - padded
    dest = pad_starts[sorted_e] + (jnp.arange(m, dtype=jnp.int32) - starts[sorted_e])
    n_blocks = -(-m // EXPERT_BLOCK) + N_EXPERTS
    p = n_blocks * EXPERT_BLOCK
    tok_buf = jnp.full((p,), t, jnp.int32).at[dest].set(flat_tok[order])
    w_buf = jnp.zeros((p,), h.dtype).at[dest].set(top_w.reshape(m)[order])
    block_start = jnp.arange(n_blocks, dtype=jnp.int32) * EXPERT_BLOCK
    block_expert = jnp.minimum(jnp.searchsorted(pad_ends, block_start, side='right'), N_EXPERTS - 1)
    h_pad = jnp.concatenate([hf, jnp.zeros((1, d), h.dtype)], axis=0)

    def expert_block(args):
        idx, e = args
        gu = h_pad[idx] @ w_gate_up[e] + b_gate_up[e]
        gate = jnp.minimum(gu[:, :D_EXPERT], SWIGLU_LIMIT)
        up = jnp.clip(gu[:, D_EXPERT:], -SWIGLU_LIMIT, SWIGLU_LIMIT)
        return ((up + 1) * gate * jax.nn.sigmoid(SWIGLU_ALPHA * gate)) @ w_down[e] + b_down[e]

    y_buf = lax.map(expert_block, (tok_buf.reshape(n_blocks, EXPERT_BLOCK), block_expert))
    y = jax.ops.segment_sum(y_buf.reshape(p, d) * w_buf[:, None], tok_buf, num_segments=t + 1)
    return y[:t].reshape(bsz, seq, d)


def setup_inputs(seed: int = 0) -> dict:
    key = jax.random.key(seed)
    keys = iter(jax.random.split(key, 40))
    f32 = jnp.float32

    def nrm(shape, scale):
        return jax.random.normal(next(keys), shape, f32) * scale

    def unif(shape, lo, hi):
        return jax.random.uniform(next(keys), shape, f32, lo, hi)

    L, D, CA, CB = DEPTH, D_MODEL, CONV_WIDTH, RWKV_WIDTH
    LV = DEPTH - 1
    return {
        'x': nrm((BATCH, SEQ, D), 1.0),
        'c': nrm((BATCH, D), 1.0),
        'norm1_g': 1.0 + nrm((L, D), 0.02),
        'norm2_g': 1.0 + nrm((L, D), 0.02),
        'w_mod': nrm((L, D, 6 * D), 0.5 * D ** -0.5),
        'b_mod': nrm((L, 6 * D), 0.02),
        'w_in': nrm((L, D, IN_COLS), D ** -0.5),
        'conv_w': nrm((L, CONV_KERNEL, 1, CA), CONV_KERNEL ** -0.5),
        'conv_b': nrm((L, CA), 0.02),
        'conv_ln_g': 1.0 + nrm((L, CA), 0.02),
        'conv_ln_b': nrm((L, CA), 0.02),
        'w_conv_proj': nrm((L, CA, D), CA ** -0.5),
        'mu_shift': unif((L, RWKV_COLS), 0.0, 1.0),
        'w0': unif((L, CB), -5.0, -0.5),
        'w2': nrm((L, R_DECAY, CB), 0.5 * R_DECAY ** -0.5),
        'a0': nrm((L, CB), 0.1),
        'a2': nrm((L, R_AAA, CB), R_AAA ** -0.5),
        'g2': nrm((L, R_GATE, CB), R_GATE ** -0.5),
        'k_k': 0.85 + nrm((L, CB), 0.05),
        'k_a': 1.0 + nrm((L, CB), 0.05),
        'r_k': nrm((L, CB), 0.1),
        'gn_g': 1.0 + nrm((L, CB), 0.02),
        'gn_b': nrm((L, CB), 0.02),
        'w_rwkv_proj': nrm((L, CB, D), CB ** -0.5),
        'v0': nrm((LV, CB), 0.1),
        'v1': nrm((LV, D, R_MV), D ** -0.5),
        'mu_v': unif((LV, R_MV), 0.0, 1.0),
        'v2': nrm((LV, R_MV, CB), R_MV ** -0.5),
        'w_out': nrm((L, D, D), D ** -0.5),
        'router_w': nrm((L, D, N_EXPERTS), D ** -0.5),
        'router_b': nrm((L, N_EXPERTS), 0.01),
        'w_gate_up': nrm((L, N_EXPERTS, D, 2 * D_EXPERT), D ** -0.5),
        'b_gate_up': nrm((L, N_EXPERTS, 2 * D_EXPERT), 0.02),
        'w_down': nrm((L, N_EXPERTS, D_EXPERT, D), D_EXPERT ** -0.5),
        'b_down': nrm((L, N_EXPERTS, D), 0.02),
        'final_g': 1.0 + nrm((D,), 0.02),
    }


def reference(x, c, norm1_g, norm2_g, w_mod, b_mod, w_in, conv_w, conv_b, conv_ln_g, conv_ln_b,
              w_conv_proj, mu_shift, w0, w2, a0, a2, g2, k_k, k_a, r_k, gn_g, gn_b, w_rwkv_proj,
              v0, v1, mu_v, v2, w_out, router_w, router_b, w_gate_up, b_gate_up, w_down, b_down,
              final_g):
    cond = jax.nn.silu(c)
    v_first = None
    c0 = 2 * CONV_WIDTH
    c1 = c0 + RWKV_COLS
    for l in range(DEPTH):
        mod = (cond @ w_mod[l] + b_mod[l])[:, None, :]
        sh1, sc1, gt1, sh2, sc2, gt2 = jnp.split(mod, 6, axis=-1)
        u = rmsnorm(x, norm1_g[l]) * (1 + sc1) + sh1
        proj = u @ w_in[l]
        y_a = conformer_conv_branch(proj[..., :c0], conv_w[l], conv_b[l], conv_ln_g[l],
                                    conv_ln_b[l], w_conv_proj[l])
        z = token_shift(proj[..., c0:c1], mu_shift[l])
        vres = None if l == 0 else (v0[l - 1], v1[l - 1], mu_v[l - 1], v2[l - 1])
        y_b, v_first = rwkv7_branch(z, u, w0[l], w2[l], a0[l], a2[l], g2[l], k_k[l], k_a[l],
                                    r_k[l], gn_g[l], gn_b[l], w_rwkv_proj[l], v_first, vres)
        gates = jax.nn.sigmoid(proj[..., c1:])
        mixed = (gates[..., :D_MODEL] * y_a + gates[..., D_MODEL:] * y_b) @ w_out[l]
        x = x + gt1 * mixed
        h = rmsnorm(x, norm2_g[l]) * (1 + sc2) + sh2
        x = x + gt2 * moe_ffn(h, router_w[l], router_b[l], w_gate_up[l], b_gate_up[l],
                              w_down[l], b_down[l])
    return rmsnorm(x, final_g)
```

```python
import numpy as np
from contextlib import ExitStack
import concourse.bass as bass
import concourse.mybir as mybir
from concourse.bass_utils import run_bass_kernel_spmd

F32 = mybir.dt.float32
BF16 = mybir.dt.bfloat16
AF = mybir.ActivationFunctionType
ALU = mybir.AluOpType
AX = mybir.AxisListType

ENGS = ("pe", "act", "dve", "pool", "sp")
RMS_EPS = 1e-5
LN_EPS = 1e-5
GN_EPS = 64e-5
SW_LIMIT = 7.0
SW_ALPHA = 1.702
EXP_M05 = float(np.exp(-0.5))


class Cfg:
    def __init__(self, D=2048, S=2048, NB=2, CA=1024, H=32, E=32, DE=1024, L=2, NCORES=8):
        self.D, self.S, self.NB, self.CA, self.H, self.E, self.DE, self.L = D, S, NB, CA, H, E, DE, L
        self.NCORES = NCORES
        self.N = 64
        self.CB = H * 64
        self.RD, self.RA, self.RG, self.RMV, self.KW = 96, 96, 256, 64, 31
        self.RW = 3 * self.CB + self.RD + self.RA + self.RG
        self.INC = 2 * CA + self.RW + 2 * D
        self.INCX = self.INC + self.RMV
        self.TOK = NB * S
        self.TT = min(512, S)
        self.C = 64
        self.c0 = 2 * CA
        self.c1 = self.c0 + self.RW


class Dep:
    __slots__ = ("w", "r")

    def __init__(self):
        self.w = None
        self.r = {}


class FW:
    def __init__(self, nc, n_dma_sems=32):
        self.nc = nc
        self.stream = {e: [] for e in ENGS}
        self.sems = {}
        self.cnt = {}
        self.seen = {e: {} for e in ENGS}
        for e in ENGS:
            self.sems["E" + e] = nc.alloc_semaphore("sem_" + e)
            self.cnt["E" + e] = 0
        self.dma_keys = []
        for i in range(n_dma_sems):
            k = "D%d" % i
            self.sems[k] = nc.alloc_semaphore("semd%d" % i)
            self.cnt[k] = 0
            self.dma_keys.append(k)
        self.dma_rr = 0

    def _need(self, eng, reads, writes):
        seen = self.seen[eng]
        st = self.stream[eng]
        if eng == "pe":
            seen["Epe"] = self.cnt["Epe"]
        for d in reads:
            if d.w is not None and seen.get(d.w[0], 0) < d.w[1]:
                seen[d.w[0]] = d.w[1]
                st.append(("wait", d.w[0], d.w[1]))
        for d in writes:
            if d.w is not None and seen.get(d.w[0], 0) < d.w[1]:
                seen[d.w[0]] = d.w[1]
                st.append(("wait", d.w[0], d.w[1]))
            for k, v in d.r.items():
                if seen.get(k, 0) < v:
                    seen[k] = v
                    st.append(("wait", k, v))

    def _mark(self, ev, reads, writes):
        k, v = ev
        for d in reads:
            if d.r.get(k, 0) < v:
                d.r[k] = v
        for d in writes:
            d.w = ev
            d.r = {}

    def op(self, eng, fn, reads=(), writes=()):
        self._need(eng, reads, writes)
        k = "E" + eng
        self.cnt[k] += 1
        ev = (k, self.cnt[k])
        self.stream[eng].append(("op", fn, k, 1))
        self._mark(ev, reads, writes)
        return ev

    def dma(self, q, fn, reads=(), writes=()):
        k = self.dma_keys[self.dma_rr % len(self.dma_keys)]
        self.dma_rr += 1
        if self.cnt[k] > 0 and self.seen[q].get(k, 0) < self.cnt[k]:
            self.seen[q][k] = self.cnt[k]
            self.stream[q].append(("wait", k, self.cnt[k]))
        self._need(q, reads, writes)
        self.cnt[k] += 16
        ev = (k, self.cnt[k])
        self.stream[q].append(("op", fn, k, 16))
        self._mark(ev, reads, writes)
        return ev

    def barrier(self):
        for e in ENGS:
            for k, v in self.cnt.items():
                if v > 0 and self.seen[e].get(k, 0) < v:
                    self.seen[e][k] = v
                    self.stream[e].append(("wait", k, v))

    def emit(self):
        nc = self.nc
        fw = self
        names = {"pe": "tensor", "act": "scalar", "dve": "vector", "pool": "gpsimd", "sp": "sync"}
        with nc.Block() as block:
            for e in ENGS:
                items = self.stream[e]

                def body(eng, items=items):
                    for it in items:
                        if it[0] == "wait":
                            eng.wait_ge(fw.sems[it[1]], it[2])
                        else:
                            it[1](eng).then_inc(fw.sems[it[2]], it[3])
                getattr(block, names[e])(body)
        self.stream = {e: [] for e in ENGS}


class Ctx:
    def __init__(self, nc, cfg):
        self.nc = nc
        self.cfg = cfg
        self.fw = FW(nc)
        self.ps = [nc.alloc_psum_tensor("psb%d" % i, [128, 512], F32).ap() for i in range(8)]
        self.psd = [Dep() for _ in range(8)]
        self.ps_rr = 0
        self.uid = 0
        self.dram = {}

    def psum(self):
        i = self.ps_rr % 8
        self.ps_rr += 1
        return self.ps[i], self.psd[i]

    def name(self, p):
        self.uid += 1
        return "%s_%d" % (p, self.uid)

    def sb(self, es, shape, dt=F32, tag="t"):
        t = es.enter_context(self.nc.sbuf_tensor(self.name(tag), list(shape), dt))
        return t.ap() if hasattr(t, "ap") else t

    def dma(self, q, out, in_, reads=(), writes=(), **kw):
        return self.fw.dma(q, lambda e: e.dma_start(out=out, in_=in_, **kw), reads=reads, writes=writes)

    def act(self, out, in_, func, reads=(), writes=(), eng="act", **kw):
        return self.fw.op("act", lambda e: e.activation(out=out, in_=in_, func=func, **kw), reads=reads, writes=writes)

    def tt(self, eng, out, in0, in1, op, reads=(), writes=()):
        return self.fw.op(eng, lambda e: e.tensor_tensor(out=out, in0=in0, in1=in1, op=op), reads=reads, writes=writes)

    def ts(self, eng, out, in0, s1, s2, op0, op1=None, reads=(), writes=()):
        if op1 is None:
            return self.fw.op(eng, lambda e: e.tensor_scalar(out=out, in0=in0, scalar1=s1, scalar2=None, op0=op0),
                              reads=reads, writes=writes)
        return self.fw.op(eng, lambda e: e.tensor_scalar(out=out, in0=in0, scalar1=s1, scalar2=s2, op0=op0, op1=op1),
                          reads=reads, writes=writes)

    def stt(self, eng, out, in0, scalar, in1, op0, op1, reads=(), writes=()):
        return self.fw.op(eng, lambda e: e.scalar_tensor_tensor(out=out, in0=in0, scalar=scalar, in1=in1, op0=op0, op1=op1),
                          reads=reads, writes=writes)

    def copy(self, eng, out, in_, reads=(), writes=()):
        if eng == "act":
            return self.fw.op("act", lambda e: e.activation(out=out, in_=in_, func=AF.Copy), reads=reads, writes=writes)
        return self.fw.op(eng, lambda e: e.tensor_copy(out=out, in_=in_), reads=reads, writes=writes)

    def mm(self, out, lhsT, rhs, start, stop, reads=(), writes=()):
        return self.fw.op("pe", lambda e: e.matmul(out, lhsT=lhsT, rhs=rhs, start=start, stop=stop),
                          reads=reads, writes=writes)

    def tr(self, out, in_, ident, reads=(), writes=()):
        return self.fw.op("pe", lambda e: e.transpose(out=out, in_=in_, identity=ident), reads=reads, writes=writes)

    def memset(self, eng, ap, val, writes=()):
        return self.fw.op(eng, lambda e: e.memset(ap, val), writes=writes)

    def end_stage(self):
        self.fw.barrier()
        import os
        if os.environ.get("KSTAT"):
            tot = {e: (sum(1 for i in v if i[0] == "op"), sum(1 for i in v if i[0] == "wait")) for e, v in self.fw.stream.items()}
            print("STAGE", tot, flush=True)
        self.fw.emit()


class Tl:
    __slots__ = ("ap", "d")

    def __init__(self, ap, d=None):
        self.ap = ap
        self.d = d if d is not None else Dep()

    def __getitem__(self, idx):
        return Tl(self.ap[idx], self.d)

    def v(self, ap):
        return Tl(ap, self.d)


def _rd(*xs):
    return [x.d for x in xs if isinstance(x, Tl)]


def _ap(x):
    return x.ap if isinstance(x, Tl) else x


class Ops:
    def __init__(self, cx):
        self.cx = cx
        self.fw = cx.fw

    def tile(self, es, shape, dt=F32, tag="t"):
        return Tl(self.cx.sb(es, shape, dt, tag))

    def pst(self):
        ap, d = self.cx.psum()
        return Tl(ap, d)

    def A(self, out, in_, func, scale=None, bias=None, accum=None):
        kw = {}
        if scale is not None:
            kw["scale"] = _ap(scale)
        if bias is not None:
            kw["bias"] = _ap(bias)
        if accum is not None:
            kw["accum_out"] = accum.ap
        o, i = out.ap, in_.ap
        wr = [out.d] + ([accum.d] if accum is not None else [])
        return self.fw.op("act", lambda e: e.activation(out=o, in_=i, func=func, **kw),
                          reads=_rd(in_, scale, bias), writes=wr)

    def T2(self, eng, out, a, b, op):
        o, x, y = out.ap, a.ap, b.ap
        return self.fw.op(eng, lambda e: e.tensor_tensor(out=o, in0=x, in1=y, op=op), reads=_rd(a, b), writes=[out.d])

    def TS(self, eng, out, a, s1, s2, op0, op1=None):
        o, x, p1, p2 = out.ap, a.ap, _ap(s1), _ap(s2)
        if op1 is None:
            return self.fw.op(eng, lambda e: e.tensor_scalar(out=o, in0=x, scalar1=p1, scalar2=None, op0=op0),
                              reads=_rd(a, s1), writes=[out.d])
        return self.fw.op(eng, lambda e: e.tensor_scalar(out=o, in0=x, scalar1=p1, scalar2=p2, op0=op0, op1=op1),
                          reads=_rd(a, s1, s2), writes=[out.d])

    def STT(self, eng, out, a, scalar, b, op0, op1):
        o, x, sc, y = out.ap, a.ap, _ap(scalar), b.ap
        eng = "dve"
        return self.fw.op(eng, lambda e: e.scalar_tensor_tensor(out=o, in0=x, scalar=sc, in1=y, op0=op0, op1=op1),
                          reads=_rd(a, scalar, b), writes=[out.d])

    def CP(self, eng, out, in_):
        o, i = out.ap, in_.ap
        if eng == "act":
            return self.fw.op("act", lambda e: e.activation(out=o, in_=i, func=AF.Copy), reads=[in_.d], writes=[out.d])
        return self.fw.op(eng, lambda e: e.tensor_copy(out=o, in_=i), reads=[in_.d], writes=[out.d])

    def RCP(self, out, in_):
        o, i = out.ap, in_.ap
        return self.fw.op("dve", lambda e: e.reciprocal(out=o, in_=i), reads=[in_.d], writes=[out.d])

    def MS(self, eng, out, val):
        o = out.ap
        return self.fw.op(eng, lambda e: e.memset(o, val), writes=[out.d])

    def MM(self, out, lhsT, rhs, start, stop):
        o, l, r = out.ap, lhsT.ap, rhs.ap
        return self.fw.op("pe", lambda e: e.matmul(o, lhsT=l, rhs=r, start=start, stop=stop),
                          reads=[lhsT.d, rhs.d], writes=[out.d])

    def TR(self, out, in_, ident):
        o, i, idn = out.ap, in_.ap, ident.ap
        return self.fw.op("pe", lambda e: e.transpose(out=o, in_=i, identity=idn), reads=[in_.d, ident.d], writes=[out.d])

    def LD(self, q, out, dram_ap):
        o = out.ap
        return self.fw.dma(q, lambda e: e.dma_start(out=o, in_=dram_ap), writes=[out.d])

    def ST(self, q, dram_ap, in_):
        i = in_.ap
        return self.fw.dma(q, lambda e: e.dma_start(out=dram_ap, in_=i), reads=[in_.d])

    def consts(self, es):
        d = self.cx.dram
        K = {}
        for nm in ("ident", "ones128", "bdones"):
            K[nm] = self.tile(es, [128, 128], F32, nm)
            self.LD("sp", K[nm], d["c_" + nm])
        return K


def load_consts(cx, es):
    d = cx.dram
    K = {}
    dep = Dep()
    for nm, shape in (("ident", [128, 128]), ("ones128", [128, 128]), ("bdones", [128, 128])):
        K[nm] = cx.sb(es, shape, F32, nm)
        cx.dma("sp", K[nm], d["c_" + nm], writes=[dep])
    K["dep"] = dep
    return K


def stage_mod(cx, l):
    cfg, d = cx.cfg, cx.dram
    D, NB = cfg.D, cfg.NB
    KC = D // 128
    CBW = 256
    with ExitStack() as es:
        cT = cx.sb(es, [128, KC, NB], F32, "cT")
        ones = cx.sb(es, [1, 128], F32, "ones1")
        brow = [cx.sb(es, [1, CBW], F32, "brow") for _ in range(2)]
        wb = [cx.sb(es, [128, KC, CBW], F32, "wmod") for _ in range(2)]
        out = cx.sb(es, [NB, 6 * D], F32, "modsb")
        dc, do, dout = Dep(), Dep(), Dep()
        dbr = [Dep(), Dep()]
        dwb = [Dep(), Dep()]
        cx.dma("sp", cT, d["cT"], writes=[dc])
        cx.act(cT, cT, AF.Silu, reads=[dc], writes=[dc])
        cx.memset("dve", ones, 1.0, writes=[do])
        nblk = 6 * D // CBW
        wview = d["w_mod"][l].rearrange("(kc p) n -> p kc n", p=128)
        for cb in range(nblk):
            s = cb % 2
            cx.dma("sp", wb[s], wview[:, :, cb * CBW:(cb + 1) * CBW], writes=[dwb[s]])
            cx.dma("sp", brow[s], d["b_mod"][l:l + 1, cb * CBW:(cb + 1) * CBW], writes=[dbr[s]])
            ps, pd = cx.psum()
            for kc in range(KC):
                cx.mm(ps[0:NB, 0:CBW], cT[:, kc, :], wb[s][:, kc, :], kc == 0, False,
                      reads=[dc, dwb[s]], writes=[pd])
            cx.mm(ps[0:NB, 0:CBW], ones[0:1, 0:NB], brow[s], False, True, reads=[do, dbr[s]], writes=[pd])
            cx.copy("dve", out[:, cb * CBW:(cb + 1) * CBW], ps[0:NB, 0:CBW], reads=[pd], writes=[dout])
        cx.dma("sp", d["mod%d" % l], out, reads=[dout])
        cx.end_stage()


class Bcast:
    def __init__(self, cx, es):
        D = cx.cfg.D
        self.GS = cx.sb(es, [128, D], F32, "GS")
        self.SH = cx.sb(es, [128, D], F32, "SH")
        self.GT = cx.sb(es, [128, D], F32, "GT")
        self.tmp = cx.sb(es, [128, D], F32, "gtmp")
        self.dg, self.ds, self.dt, self.dtmp = Dep(), Dep(), Dep(), Dep()

    def build(self, cx, l, b, which):
        cfg, d = cx.cfg, cx.dram
        D = cfg.D
        off = 0 if which == 1 else 3 * D
        mod = d["mod%d" % l]
        gname = "norm1_g" if which == 1 else "norm2_g"
        cx.dma("sp", self.SH, mod[b:b + 1, off:off + D].partition_broadcast(128), writes=[self.ds])
        cx.dma("sp", self.GS, mod[b:b + 1, off + D:off + 2 * D].partition_broadcast(128), writes=[self.dg])
        cx.dma("sp", self.GT, mod[b:b + 1, off + 2 * D:off + 3 * D].partition_broadcast(128), writes=[self.dt])
        cx.dma("sp", self.tmp, d[gname][l:l + 1, :].partition_broadcast(128), writes=[self.dtmp])
        cx.stt("dve", self.GS, self.GS, 1.0, self.tmp, ALU.add, ALU.mult, reads=[self.dtmp], writes=[self.dg])


def norm_mod_tile(cx, xt, dx, ut, du, junk, dj, small, dsm, GS, dgs, SH, dsh, D):
    ss, sq, rs = small[:, 0:1], small[:, 1:2], small[:, 2:3]
    cx.fw.op("act", lambda e: e.activation(out=junk, in_=xt, func=AF.Square, accum_out=ss),
             reads=[dx], writes=[dj, dsm])
    cx.ts("dve", sq, ss, 1.0 / D, RMS_EPS, ALU.mult, ALU.add, reads=[dsm], writes=[dsm])
    cx.act(sq, sq, AF.Sqrt, reads=[dsm], writes=[dsm])
    cx.fw.op("dve", lambda e: e.reciprocal(out=rs, in_=sq), reads=[dsm], writes=[dsm])
    cx.stt("dve", ut, xt, rs, GS, ALU.mult, ALU.mult, reads=[dx, dsm, dgs], writes=[du])
    if SH is not None:
        cx.tt("pool", ut, ut, SH, ALU.add, reads=[dsh], writes=[du])


def transpose_to_fm(cx, K, ut, du, KC, dsts):
    for k0 in range(0, KC, 4):
        n = min(4, KC - k0)
        ps, pd = cx.psum()
        for j in range(n):
            cx.tr(ps[:, j * 128:(j + 1) * 128], ut[:, (k0 + j) * 128:(k0 + j + 1) * 128], K["ident"],
                  reads=[du, K["dep"]], writes=[pd])
        src = ps[:, 0:n * 128].rearrange("p (a b) -> p a b", a=n)
        for (dst, dd, eng) in dsts:
            if eng == "alt":
                eng = "act" if (k0 // 4) % 2 == 0 else "dve"
            cx.copy(eng, dst[:, k0:k0 + n, :], src, reads=[pd], writes=[dd])


def stage_inproj(cx, l, xin):
    cfg, d = cx.cfg, cx.dram
    D, S, NB, TT = cfg.D, cfg.S, cfg.NB, cfg.TT
    KC = D // 128
    NCOL = cfg.INCX
    W = d["w_inx"][l].rearrange("(kc p) n -> p kc n", p=128)
    projT = d["projT"]
    x = d[xin]
    with ExitStack() as es:
        K = load_consts(cx, es)
        xt = [cx.sb(es, [128, D], F32, "xt") for _ in range(2)]
        dxt = [Dep(), Dep()]
        ut = cx.sb(es, [128, D], F32, "ut")
        du = Dep()
        junk = cx.sb(es, [128, D], F32, "junk")
        dj = Dep()
        small = cx.sb(es, [128, 4], F32, "small")
        dsm = Dep()
        uT = cx.sb(es, [128, KC, TT], BF16, "uT")
        duT = Dep()
        wb = [cx.sb(es, [128, KC, 512], BF16, "wb") for _ in range(2)]
        dwb = [Dep(), Dep()]
        stg = [cx.sb(es, [128, TT], F32, "stg") for _ in range(4)]
        dstg = [Dep() for _ in range(4)]
        nst = 0
        nw = 0
        bc = Bcast(cx, es)
        for b in range(NB):
            if True:
                bc.build(cx, l, b, 1)
                GS, dgs, SH, dsh = bc.GS, bc.dg, bc.SH, bc.ds
                for st in range(S // TT):
                    tok0 = b * S + st * TT
                    for tt in range(TT // 128):
                        s = tt % 2
                        r0 = tok0 + tt * 128
                        cx.dma("sp", xt[s], x[r0:r0 + 128, :], writes=[dxt[s]])
                        norm_mod_tile(cx, xt[s], dxt[s], ut, du, junk, dj, small, dsm, GS, dgs, SH, dsh, D)
                        transpose_to_fm(cx, K, ut, du, KC,
                                        [(uT[:, :, tt * 128:(tt + 1) * 128], duT, "act" if tt % 2 else "dve")])
                    for c0 in range(0, NCOL, 512):
                        cw = min(512, NCOL - c0)
                        s = nw % 2
                        nw += 1
                        cx.dma("pool", wb[s][:, :, 0:cw], W[:, :, c0:c0 + cw], writes=[dwb[s]])
                        for s0 in range(0, cw, 128):
                            nsz = min(128, cw - s0)
                            ps, pd = cx.psum()
                            for kc in range(KC):
                                cx.mm(ps[0:nsz, 0:TT], wb[s][:, kc, s0:s0 + nsz], uT[:, kc, :], kc == 0, kc == KC - 1,
                                      reads=[dwb[s], duT], writes=[pd])
                            q = nst % 4
                            nst += 1
                            cx.copy("act" if q % 2 else "dve", stg[q][0:nsz, :], ps[0:nsz, 0:TT], reads=[pd], writes=[dstg[q]])
                            cx.dma("sp", projT[c0 + s0:c0 + s0 + nsz, tok0:tok0 + TT], stg[q][0:nsz, :], reads=[dstg[q]])
        cx.end_stage()


def dram_specs(cfg):
    c = cfg
    L, D, CA, CB, E, DE, NB, TOK = c.L, c.D, c.CA, c.CB, c.E, c.DE, c.NB, c.TOK
    KC, CAB, CBB = D // 128, CA // 128, CB // 128
    BLK, NBLK, NJ = moe_dims(cfg)
    NPG, DW, NPD = moe_pieces(cfg)
    NTL = TOK // 128
    ins = [
        ("x", [TOK, D]), ("cT", [128, KC, NB]),
        ("norm1_g", [L, D]), ("norm2_g", [L, D]), ("final_g", [1, D]),
        ("w_mod", [L, D, 6 * D]), ("b_mod", [L, 6 * D]),
        ("w_inx", [L, D, c.INCX]),
        ("conv_wT", [L, 128, CAB, c.KW]), ("conv_b", [L, 128, CAB]), ("conv_ln_g", [L, 128, CAB]),
        ("conv_ln_b", [L, 128, CAB]), ("w_conv_proj", [L, CA, D]),
        ("mu_rkv", [L, 128, 3 * CBB]), ("mu_w", [L, c.RD, 1]), ("mu_a", [L, c.RA, 1]), ("mu_g", [L, 128, c.RG // 128]),
        ("w0", [L, 128, CBB]), ("a0", [L, 128, CBB]), ("k_k", [L, 128, CBB]), ("k_a", [L, 128, CBB]),
        ("r_k", [L, 128, CBB]), ("gn_g", [L, 64, c.H]), ("gn_b", [L, 64, c.H]),
        ("w2", [L, c.RD, CB]), ("a2", [L, c.RA, CB]), ("g2", [L, c.RG, CB]),
        ("v0", [L, 128, CBB]), ("mu_v", [L, c.RMV, 1]), ("v2", [L, c.RMV, CB]),
        ("w_rwkv_proj", [L, CB, D]), ("w_out", [L, D, D]),
        ("router_w", [L, D, E]), ("router_b", [L, 1, E]),
        ("c_ident", [128, 128]), ("c_ones128", [128, 128]), ("c_bdones", [128, 128]),
        ("c_masks", [64, 4, 8, 64]), ("c_reset", [128, 512]),
        ("c_lstrict", [128, 128]), ("c_thr", [128, E, NJ]), ("c_blkstart", [128, NBLK, E]),
        ("c_guoff", [128, NPG]), ("c_doff", [128, NPD]), ("c_boff", [128, 2]), ("c_bmul", [128, 2]),
    ]
    for l in range(L):
        ins += [("w_gu3_%d" % l, [E * 128 * NPG, KC * 256]), ("w_d3_%d" % l, [E * 128 * NPD, (DE // 128) * DW]),
                ("b_gu2_%d" % l, [E * 128, 2 * DE // 128]), ("b_d2_%d" % l, [E, D])]
    scr = [("mod%d" % l, [NB, 6 * D], F32) for l in range(L)]
    scr += [("projT", [c.INCX, TOK], F32), ("mixAT", [D, TOK], F32)]
    scr += [(n, [CB, TOK], BF16) for n in ("sAt", "sRt", "sBt", "sKt", "sBh", "sKh", "sV")]
    scr += [(n, [CB, TOK], F32) for n in ("sbv", "sg", "vfirstT", "ybT")]
    scr += [("sPC", [CB, TOK // 64], F32)]
    scr += [("x%d" % i, [TOK, D], F32) for i in range(1, 2 * L + 1)]
    scr += [("out", [TOK, D], F32), ("Hd", [TOK, D], F32), ("Hsorted", [NBLK * BLK, D], F32),
            ("Ybuf", [NBLK * BLK, D], F32), ("idx_gu", [128, NBLK, NPG], I32),
            ("idx_d", [128, NBLK, NPD], I32), ("idx_b", [128, NBLK, 2], I32),
            ("w4", [128, NTL, 4], F32), ("d4", [128, NTL, 4], I32)]
    return [(n, s, F32, "in") for n, s in ins] + [(n, s, dt, "scratch") for n, s, dt in scr]


def build_program(cfg, stages, outputs=("out",), ext_inputs=()):
    nc = bass.Bass("TRN2", target_bir_lowering=False)
    cx = Ctx(nc, cfg)
    for n, s, dt, role in dram_specs(cfg):
        if role == "in" or n in ext_inputs:
            kind = "ExternalInput"
        elif n in outputs:
            kind = "ExternalOutput"
        else:
            kind = "Internal"
        cx.dram[n] = nc.dram_tensor(n, list(s), dt, kind=kind).ap()
    for st in stages:
        st(cx)
    return nc


def host_prep(cfg, inp, core):
    c = cfg
    L, D, CA, CB, NB = c.L, c.D, c.CA, c.CB, c.NB
    KC, CAB, CBB = D // 128, CA // 128, CB // 128
    f = lambda a: np.ascontiguousarray(a, dtype=np.float32)
    b0 = core * NB
    m = {}
    m["x"] = f(inp["x"][b0:b0 + NB].reshape(NB * c.S, D))
    m["cT"] = f(inp["c"][b0:b0 + NB].T.reshape(KC, 128, NB).transpose(1, 0, 2))
    for k in ("norm1_g", "norm2_g", "w_mod", "b_mod", "w_conv_proj", "w2", "a2", "g2", "w_rwkv_proj", "w_out",
              "router_w"):
        m[k] = f(inp[k])
    m["final_g"] = f(inp["final_g"].reshape(1, D))
    v1p = np.zeros((L, D, c.RMV), np.float32)
    v1p[1:] = inp["v1"]
    m["w_inx"] = f(np.concatenate([inp["w_in"], v1p], axis=2))
    m["conv_wT"] = f(inp["conv_w"][:, :, 0, :].transpose(0, 2, 1).reshape(L, CAB, 128, c.KW).transpose(0, 2, 1, 3))
    pc = lambda a, nb: f(a.reshape(L, nb, 128).transpose(0, 2, 1))
    for k in ("conv_b", "conv_ln_g", "conv_ln_b"):
        m[k] = pc(inp[k], CAB)
    mu = inp["mu_shift"]
    m["mu_rkv"] = f(mu[:, :3 * CB].reshape(L, 3 * CBB, 128).transpose(0, 2, 1))
    o = 3 * CB
    m["mu_w"] = f(mu[:, o:o + c.RD].reshape(L, c.RD, 1))
    m["mu_a"] = f(mu[:, o + c.RD:o + c.RD + c.RA].reshape(L, c.RA, 1))
    m["mu_g"] = f(mu[:, o + c.RD + c.RA:].reshape(L, c.RG // 128, 128).transpose(0, 2, 1))
    for k in ("w0", "a0", "k_k", "k_a", "r_k"):
        m[k] = pc(inp[k], CBB)
    for k in ("gn_g", "gn_b"):
        m[k] = f(inp[k].reshape(L, c.H, 64).transpose(0, 2, 1))
    z = lambda a: np.concatenate([np.zeros((1,) + a.shape[1:], np.float32), a], axis=0)
    m["v0"] = pc(z(inp["v0"]), CBB)
    m["mu_v"] = f(z(inp["mu_v"]).reshape(L, c.RMV, 1))
    m["v2"] = f(z(inp["v2"]))
    m["router_b"] = f(inp["router_b"].reshape(L, 1, c.E))
    E, DE = c.E, c.DE
    HW = DE // 2
    NPG, DW, NPD = moe_pieces(c)
    FC = DE // 128
    for l in range(L):
        w = inp["w_gate_up"][l]
        g_ = w[..., :DE].reshape(E, KC, 128, NPG, 128)
        u_ = w[..., DE:].reshape(E, KC, 128, NPG, 128)
        gu_ = np.stack([g_, u_], axis=4)
        m["w_gu3_%d" % l] = f(gu_.transpose(0, 2, 3, 1, 4, 5).reshape(E * 128 * NPG, KC * 256))
        wd_ = inp["w_down"][l].reshape(E, FC, 128, NPD, DW)
        m["w_d3_%d" % l] = f(wd_.transpose(0, 2, 3, 1, 4).reshape(E * 128 * NPD, FC * DW))
        m["b_gu2_%d" % l] = f(inp["b_gate_up"][l].reshape(E, 2 * DE // 128, 128).transpose(0, 2, 1).reshape(E * 128, 2 * DE // 128))
        m["b_d2_%d" % l] = f(inp["b_down"][l])
    BLK, NBLK, NJ = moe_dims(c)
    pp = np.arange(128)
    m["c_lstrict"] = (pp[:, None] < pp[None, :]).astype(np.float32)
    m["c_thr"] = f(np.broadcast_to((np.arange(NJ) * BLK)[None, None, :], (128, E, NJ)))
    m["c_blkstart"] = f(np.broadcast_to((np.arange(NBLK) * BLK)[None, :, None], (128, NBLK, E)))
    m["c_guoff"] = f(pp[:, None] * NPG + np.arange(NPG)[None, :])
    m["c_doff"] = f(pp[:, None] * NPD + np.arange(NPD)[None, :])
    m["c_boff"] = f(np.stack([pp, np.zeros(128)], axis=1))
    m["c_bmul"] = f(np.stack([np.full(128, 128.0), np.ones(128)], axis=1))
    m["c_ident"] = np.eye(128, dtype=np.float32)
    m["c_ones128"] = np.ones((128, 128), np.float32)
    bd = np.zeros((128, 128), np.float32)
    bd[:64, :64] = 1
    bd[64:, 64:] = 1
    m["c_bdones"] = bd
    s_ = np.arange(64)[:, None]
    t_ = np.arange(64)[None, :]
    masks = np.stack([(s_ < t_), (s_ > t_), (s_ <= t_), (s_ == t_)]).astype(np.float32)
    m["c_masks"] = f(np.broadcast_to(masks[:, :, None, :], (4, 64, 8, 64)).transpose(1, 0, 2, 3))
    rs = np.ones((128, 512), np.float32)
    rs[:, ::64] = 0
    m["c_reset"] = rs
    return m


def stage_conv(cx, l):
    cfg, d = cx.cfg, cx.dram
    D, S, NB, TT, CA, KW = cfg.D, cfg.S, cfg.NB, cfg.TT, cfg.CA, cfg.KW
    CAB = CA // 128
    HL = KW - 1
    projT = d["projT"]
    gA0 = cfg.c1
    with ExitStack() as es:
        K = load_consts(cx, es)
        cw = cx.sb(es, [128, CAB, KW], F32, "cw")
        cb_ = cx.sb(es, [128, CAB], F32, "cb")
        lg = cx.sb(es, [128, CAB], F32, "lg")
        lb = cx.sb(es, [128, CAB], F32, "lb")
        dpar = Dep()
        cx.dma("sp", cw, d["conv_wT"][l], writes=[dpar])
        cx.dma("sp", cb_, d["conv_b"][l], writes=[dpar])
        cx.dma("sp", lg, d["conv_ln_g"][l], writes=[dpar])
        cx.dma("sp", lb, d["conv_ln_b"][l], writes=[dpar])
        Wp = cx.sb(es, [128, CAB, D], BF16, "Wp")
        dWp = Dep()
        wv = d["w_conv_proj"][l].rearrange("(kc p) n -> p kc n", p=128)
        for kc in range(CAB):
            cx.dma("pool", Wp[:, kc, :], wv[:, kc, :], writes=[dWp])
        a_t = [cx.sb(es, [128, HL + TT], F32, "a_t") for _ in range(2)]
        g_t = [cx.sb(es, [128, HL + TT], F32, "g_t") for _ in range(2)]
        da = [Dep(), Dep()]
        dg = [Dep(), Dep()]
        accs = [cx.sb(es, [128, CAB, TT], F32, "acc") for _ in range(2)]
        sqs = [cx.sb(es, [128, CAB, TT], F32, "sq") for _ in range(2)]
        daccs = [[Dep() for _ in range(CAB)] for _ in range(2)]
        dsqs = [[Dep() for _ in range(CAB)] for _ in range(2)]
        ntile = 0
        mean = cx.sb(es, [128, TT], F32, "mean")
        rstd = cx.sb(es, [128, TT], F32, "rstd")
        dmean, drstd = Dep(), Dep()
        cT = cx.sb(es, [128, CAB, TT], BF16, "cT")
        dcT = Dep()
        gat = [cx.sb(es, [128, TT], F32, "gat") for _ in range(2)]
        dgat = [Dep(), Dep()]
        stg = [cx.sb(es, [128, TT], F32, "stg") for _ in range(2)]
        dstg = [Dep(), Dep()]
        n = 0
        for b in range(NB):
            for st in range(S // TT):
                t0 = st * TT
                tok0 = b * S + t0
                acc, sq, dacc, dsq = accs[ntile % 2], sqs[ntile % 2], daccs[ntile % 2], dsqs[ntile % 2]
                ntile += 1
                for cb in range(CAB):
                    s = n % 2
                    n += 1
                    if t0 == 0:
                        cx.memset("pool", a_t[s][:, 0:HL], 0.0, writes=[da[s]])
                        cx.memset("pool", g_t[s][:, 0:HL], 0.0, writes=[dg[s]])
                        cx.dma("sp", a_t[s][:, HL:], projT[cb * 128:(cb + 1) * 128, tok0:tok0 + TT], writes=[da[s]])
                        cx.dma("sp", g_t[s][:, HL:], projT[CA + cb * 128:CA + (cb + 1) * 128, tok0:tok0 + TT], writes=[dg[s]])
                    else:
                        cx.dma("sp", a_t[s], projT[cb * 128:(cb + 1) * 128, tok0 - HL:tok0 + TT], writes=[da[s]])
                        cx.dma("sp", g_t[s], projT[CA + cb * 128:CA + (cb + 1) * 128, tok0 - HL:tok0 + TT], writes=[dg[s]])
                    cx.act(g_t[s], g_t[s], AF.Sigmoid, reads=[dg[s]], writes=[dg[s]])
                    cx.tt("pool", a_t[s], a_t[s], g_t[s], ALU.mult, reads=[dg[s]], writes=[da[s]])
                    o = acc[:, cb, :]
                    cx.ts("dve", o, a_t[s][:, 0:TT], cw[:, cb, 0:1], cb_[:, cb:cb + 1], ALU.mult, ALU.add,
                          reads=[da[s], dpar], writes=[dacc[cb]])
                    for k in range(1, KW):
                        cx.stt("dve", o, a_t[s][:, k:k + TT], cw[:, cb, k:k + 1], o, ALU.mult, ALU.add,
                               reads=[da[s]], writes=[dacc[cb]])
                    cx.act(sq[:, cb, :], o, AF.Square, reads=[dacc[cb]], writes=[dsq[cb]])
                ps1, pd1 = cx.psum()
                for cb in range(CAB):
                    cx.mm(ps1[:, 0:TT], K["ones128"], acc[:, cb, :], cb == 0, cb == CAB - 1,
                          reads=[K["dep"], dacc[cb]], writes=[pd1])
                ps2, pd2 = cx.psum()
                for cb in range(CAB):
                    cx.mm(ps2[:, 0:TT], K["ones128"], sq[:, cb, :], cb == 0, cb == CAB - 1,
                          reads=[dsq[cb]], writes=[pd2])
                cx.fw.op("act", lambda e, ps1=ps1: e.mul(out=mean, in_=ps1[:, 0:TT], mul=1.0 / CA), reads=[pd1], writes=[dmean])
                cx.tt("dve", rstd, mean, mean, ALU.mult, reads=[dmean], writes=[drstd])
                cx.stt("dve", rstd, ps2[:, 0:TT], 1.0 / CA, rstd, ALU.mult, ALU.subtract, reads=[pd2], writes=[drstd])
                cx.ts("dve", rstd, rstd, LN_EPS, None, ALU.add, reads=[], writes=[drstd])
                cx.act(rstd, rstd, AF.Sqrt, reads=[drstd], writes=[drstd])
                cx.fw.op("dve", lambda e: e.reciprocal(out=rstd, in_=rstd), reads=[drstd], writes=[drstd])
                for cb in range(CAB):
                    o = acc[:, cb, :]
                    eng = "dve" if cb % 2 == 0 else "pool"
                    cx.tt(eng, o, o, mean, ALU.subtract, reads=[dmean], writes=[dacc[cb]])
                    cx.tt(eng, o, o, rstd, ALU.mult, reads=[drstd], writes=[dacc[cb]])
                    cx.act(cT[:, cb, :], o, AF.Silu, reads=[dacc[cb], dpar], writes=[dcT],
                           scale=lg[:, cb:cb + 1], bias=lb[:, cb:cb + 1])
                for nb in range(D // 128):
                    s = nb % 2
                    cx.dma("sp", gat[s], projT[gA0 + nb * 128:gA0 + (nb + 1) * 128, tok0:tok0 + TT], writes=[dgat[s]])
                    cx.act(gat[s], gat[s], AF.Sigmoid, reads=[dgat[s]], writes=[dgat[s]])
                    ps, pd = cx.psum()
                    for kc in range(CAB):
                        cx.mm(ps[:, 0:TT], Wp[:, kc, nb * 128:(nb + 1) * 128], cT[:, kc, :], kc == 0, kc == CAB - 1,
                              reads=[dWp, dcT], writes=[pd])
                    cx.tt("dve", stg[s], ps[:, 0:TT], gat[s], ALU.mult, reads=[pd, dgat[s]], writes=[dstg[s]])
                    cx.dma("sp", d["mixAT"][nb * 128:(nb + 1) * 128, tok0:tok0 + TT], stg[s], reads=[dstg[s]])
        cx.end_stage()


def stage_rwkv_prep(cx, l):
    cfg, d = cx.cfg, cx.dram
    o = Ops(cx)
    D, S, NB, TT, CB = cfg.D, cfg.S, cfg.NB, cfg.TT, cfg.CB
    CBB = CB // 128
    C = cfg.C
    NCH = TT // C
    RD, RA, RG, RMV = cfg.RD, cfg.RA, cfg.RG, cfg.RMV
    projT = d["projT"]
    c0 = cfg.c0
    with ExitStack() as es:
        K = o.consts(es)
        reset = o.tile(es, [128, TT], F32, "reset")
        o.LD("sp", reset, d["c_reset"][:, 0:TT])
        mu_rkv = o.tile(es, [128, 3 * CBB], F32, "mu_rkv")
        o.LD("sp", mu_rkv, d["mu_rkv"][l])
        mu_w = o.tile(es, [RD, 1], F32, "mu_w")
        o.LD("sp", mu_w, d["mu_w"][l])
        mu_a = o.tile(es, [RA, 1], F32, "mu_a")
        o.LD("sp", mu_a, d["mu_a"][l])
        mu_g = o.tile(es, [128, RG // 128], F32, "mu_g")
        o.LD("sp", mu_g, d["mu_g"][l])
        mu_v = o.tile(es, [RMV, 1], F32, "mu_v")
        o.LD("sp", mu_v, d["mu_v"][l])
        vecs = {}
        for nm in ("w0", "a0", "k_k", "k_a", "r_k", "v0"):
            vecs[nm] = o.tile(es, [128, CBB], F32, nm)
            o.LD("sp", vecs[nm], d[nm][l])
        omka = o.tile(es, [128, CBB], F32, "omka")
        o.TS("dve", omka, vecs["k_a"], -1.0, 1.0, ALU.mult, ALU.add)
        w2 = o.tile(es, [RD, CB], F32, "w2")
        o.LD("sp", w2, d["w2"][l])
        a2 = o.tile(es, [RA, CB], F32, "a2")
        o.LD("sp", a2, d["a2"][l])
        g2 = o.tile(es, [128, RG // 128, CB], F32, "g2")
        o.LD("sp", g2, d["g2"][l].rearrange("(j p) n -> p j n", p=128))
        v2 = o.tile(es, [RMV, CB], F32, "v2")
        o.LD("sp", v2, d["v2"][l])

        def T(tag, rows=128, cols=TT):
            return o.tile(es, [rows, cols], F32, tag)

        raw = [T("raw%d" % i, 128, TT + 1) for i in range(6)]
        nraw = [0]

        def shifted(out, row0, rows, tok0, first, mu, eng):
            rw = raw[nraw[0] % 6][0:rows]
            nraw[0] += 1
            if first:
                o.MS("pool", rw[:, 0:1], 0.0)
                o.LD("sp", rw[:, 1:], projT[row0:row0 + rows, tok0:tok0 + TT])
            else:
                o.LD("sp", rw, projT[row0:row0 + rows, tok0 - 1:tok0 + TT])
            o.T2(eng, out, rw[:, 0:TT], rw[:, 1:TT + 1], ALU.subtract)
            o.STT(eng, out, out, mu, rw[:, 1:TT + 1], ALU.mult, ALU.add)

        tw, zaT, zvT = T("tw", RD), T("zaT", RA), T("zvT", RMV)
        sgz = o.tile(es, [128, RG // 128, TT], F32, "sgz")

        def make_set():
            W_ = {nm: T(nm) for nm in ("r", "k", "v", "lw", "a", "kk", "tmp", "tmp2", "keff", "bs",
                                       "G", "Gx", "eG", "eGx", "enG", "eH", "vf")}
            W_["PC"] = o.tile(es, [128, NCH], F32, "PC")
            outs_ = {nm: o.tile(es, [128, TT], BF16, "o_" + nm) for nm in ("sAt", "sRt", "sBt", "sKt", "sBh", "sKh", "sV")}
            outs_.update({nm: T("o_" + nm) for nm in ("sbv", "sg")})
            W_["outs"] = outs_
            return W_

        wsets = [make_set(), make_set()]
        for b in range(NB):
            for st in range(S // TT):
                tok0 = b * S + st * TT
                first = st == 0
                ro = c0 + 3 * CB
                shifted(tw, ro, RD, tok0, first, mu_w[:, 0:1], "dve")
                o.A(tw, tw, AF.Tanh)
                shifted(zaT, ro + RD, RA, tok0, first, mu_a[:, 0:1], "pool")
                for j in range(RG // 128):
                    shifted(sgz[:, j, :], ro + RD + RA + j * 128, 128, tok0, first, mu_g[:, j:j + 1], "dve")
                o.A(sgz, sgz, AF.Sigmoid)
                if l > 0:
                    shifted(zvT, cfg.INC, RMV, tok0, first, mu_v[:, 0:1], "pool")
                def prep_cb(cb, W_):
                    cs = slice(cb * 128, (cb + 1) * 128)
                    col = lambda t, j=cb: t[:, j:j + 1]
                    r, k, v, lw, a, kk, tmp, tmp2, keff, bs = (W_[n_] for n_ in ("r", "k", "v", "lw", "a", "kk", "tmp", "tmp2", "keff", "bs"))
                    G, Gx, eG, eGx, enG, eH, vf, PC, outs = (W_[n_] for n_ in ("G", "Gx", "eG", "eGx", "enG", "eH", "vf", "PC", "outs"))
                    shifted(r, c0 + cb * 128, 128, tok0, first, mu_rkv[:, cb:cb + 1], "dve")
                    shifted(k, c0 + CB + cb * 128, 128, tok0, first, mu_rkv[:, CBB + cb:CBB + cb + 1], "pool")
                    shifted(v, c0 + 2 * CB + cb * 128, 128, tok0, first, mu_rkv[:, 2 * CBB + cb:2 * CBB + cb + 1], "dve")
                    ps = o.pst()
                    o.MM(ps[:, 0:TT], w2[:, cs], tw, True, True)
                    o.A(lw, ps[:, 0:TT], AF.Sigmoid, bias=col(vecs["w0"]))
                    o.TS("pool", lw, lw, -EXP_M05, None, ALU.mult)
                    yield
                    ps = o.pst()
                    o.MM(ps[:, 0:TT], a2[:, cs], zaT, True, True)
                    o.A(a, ps[:, 0:TT], AF.Sigmoid, bias=col(vecs["a0"]))
                    yield
                    ps = o.pst()
                    nj = RG // 128
                    for j in range(nj):
                        o.MM(ps[:, 0:TT], g2[:, j, cs], sgz[:, j, :], j == 0, j == nj - 1)
                    o.CP("act", outs["sg"], ps[:, 0:TT])
                    o.ST("sp", d["sg"][cs, tok0:tok0 + TT], outs["sg"])
                    yield
                    if l == 0:
                        o.ST("sp", d["vfirstT"][cs, tok0:tok0 + TT], v)
                    else:
                        ps = o.pst()
                        o.MM(ps[:, 0:TT], v2[:, cs], zvT, True, True)
                        o.A(tmp, ps[:, 0:TT], AF.Sigmoid, bias=col(vecs["v0"]))
                        o.LD("sp", vf, d["vfirstT"][cs, tok0:tok0 + TT])
                        o.T2("pool", vf, vf, v, ALU.subtract)
                        o.T2("pool", vf, vf, tmp, ALU.mult)
                        o.T2("pool", v, v, vf, ALU.add)
                    o.CP("act", outs["sV"], v)
                    o.ST("sp", d["sV"][cs, tok0:tok0 + TT], outs["sV"])
                    yield
                    o.TS("dve", kk, k, col(vecs["k_k"]), None, ALU.mult)
                    o.T2("pool", tmp, kk, kk, ALU.mult)
                    ps = o.pst()
                    o.MM(ps[:, 0:TT], K["bdones"], tmp, True, True)
                    o.A(tmp2, ps[:, 0:TT], AF.Sqrt)
                    o.TS("dve", tmp2, tmp2, 1e-12, None, ALU.max)
                    o.RCP(tmp2, tmp2)
                    o.T2("dve", kk, kk, tmp2, ALU.mult)
                    yield
                    o.TS("pool", tmp, a, col(vecs["k_a"]), col(omka), ALU.mult, ALU.add)
                    o.T2("pool", keff, k, tmp, ALU.mult)
                    o.T2("dve", bs, kk, a, ALU.mult)
                    o.STT("dve", tmp, r, col(vecs["r_k"]), keff, ALU.mult, ALU.mult)
                    ps = o.pst()
                    o.MM(ps[:, 0:TT], K["bdones"], tmp, True, True)
                    o.T2("dve", outs["sbv"], ps[:, 0:TT], v, ALU.mult)
                    o.ST("sp", d["sbv"][cs, tok0:tok0 + TT], outs["sbv"])
                    yield
                    Ga, lwa, rsa = G.ap, lw.ap, reset.ap
                    cx.fw.op("dve", lambda e, Ga=Ga, lwa=lwa, rsa=rsa: e.tensor_tensor_scan(
                        out=Ga, data0=rsa, data1=lwa, initial=0.0, op0=ALU.mult, op1=ALU.add),
                        reads=[lw.d, reset.d], writes=[G.d])
                    o.T2("pool", Gx, G, lw, ALU.subtract)
                    o.A(eG, G, AF.Exp)
                    o.A(eGx, Gx, AF.Exp)
                    o.A(enG, G, AF.Exp, scale=-1.0)
                    yield
                    G3 = G.ap.rearrange("p (c t) -> p c t", t=C)
                    GCb = G.v(G3[:, :, C - 1:C].to_broadcast([128, NCH, C]))
                    o.T2("dve", tmp.v(tmp.ap.rearrange("p (c t) -> p c t", t=C)), GCb, G.v(G3), ALU.subtract)
                    o.A(eH, tmp, AF.Exp)
                    o.A(PC.v(PC.ap.rearrange("p (c o) -> p c o", o=1)), G.v(G3[:, :, C - 1:C]), AF.Exp)
                    ch0 = tok0 // C
                    o.ST("sp", d["sPC"][cs, ch0:ch0 + NCH], PC)
                    yield
                    o.STT("dve", outs["sAt"], kk, -1.0, eGx, ALU.mult, ALU.mult)
                    o.T2("pool", outs["sRt"], r, eG, ALU.mult)
                    o.T2("dve", outs["sBt"], bs, enG, ALU.mult)
                    o.T2("pool", outs["sKt"], keff, enG, ALU.mult)
                    o.T2("dve", outs["sBh"], bs, eH, ALU.mult)
                    o.T2("pool", outs["sKh"], keff, eH, ALU.mult)
                    for nm in ("sAt", "sRt", "sBt", "sKt", "sBh", "sKh"):
                        o.ST("sp", d[nm][cs, tok0:tok0 + TT], outs[nm])

                for cb0 in range(0, CBB, 2):
                    gens = [prep_cb(cb0 + i, wsets[i]) for i in range(2) if cb0 + i < CBB]
                    live = list(gens)
                    while live:
                        for gn in list(live):
                            try:
                                next(gn)
                            except StopIteration:
                                live.remove(gn)
        cx.end_stage()


def stage_rwkv_scan(cx, l):
    cfg, d = cx.cfg, cx.dram
    o = Ops(cx)
    S, NB, CB, H = cfg.S, cfg.NB, cfg.CB, cfg.H
    C = cfg.C
    GH = min(8, H)
    NG = NB * H // GH
    SCT = C
    W = GH * C
    names_b = ("sAt", "sRt", "sBt", "sKt", "sBh", "sKh", "sV")
    names_f = ("sbv", "sg")
    names = names_b + names_f
    with ExitStack() as es:
        K = o.consts(es)
        ones64 = K["ones128"][0:64, 0:64]
        id64b = o.tile(es, [64, 64], BF16, "id64b")
        o.CP("dve", id64b, K["ident"][0:64, 0:64])
        masks = o.tile(es, [64, 4, GH, C], F32, "masks")
        o.LD("sp", masks, d["c_masks"][:, :, 0:GH, :])
        Msu, Msl, Miu, Meye = (masks[:, i] for i in range(4))
        gng = o.tile(es, [64, H], F32, "gng")
        gnb = o.tile(es, [64, H], F32, "gnb")
        o.LD("sp", gng, d["gn_g"][l])
        o.LD("sp", gnb, d["gn_b"][l])
        def flat(t):
            return t.v(t.ap.rearrange("p h c -> p (h c)"))

        def ps3(ps):
            return ps.v(ps.ap[0:64, 0:W].rearrange("p (h c) -> p h c", h=GH))

        def permm(outps, lhs_fn, rhs_fn, first=True, last=True):
            for h in range(GH):
                o.MM(outps[0:64, h * C:(h + 1) * C], lhs_fn(h), rhs_fn(h), first, last)

        def G3(tag, dt=F32):
            return o.tile(es, [64, GH, C], dt, tag)

        def make_slot():
            t = {}
            t["inb"] = [{nm: o.tile(es, [64, GH, SCT], BF16 if nm in names_b else F32, "in_" + nm) for nm in names}
                        for _ in range(2)]
            t["PCt"] = o.tile(es, [64, GH, S // C], F32, "PCt")
            for nm in ("Vtok", "Bhtok", "Khtok", "Aak", "Arb", "Ark", "WT", "UT"):
                t[nm] = G3(nm, BF16)
            t["P"] = [G3("P0", BF16), G3("P1", BF16)]
            t["PT"] = [G3("PT0", BF16), G3("PT1", BF16)]
            t["Tm"] = [G3("T0"), G3("T1")]
            t["Tb"] = [G3("Tb0", BF16), G3("Tb1", BF16)]
            for nm in ("Y", "Ysq", "mean", "rstd", "yo"):
                t[nm] = G3(nm)
            t["Sst"] = [G3("S0"), G3("S1")]
            t["Sb"] = [G3("Sb0", BF16), G3("Sb1", BF16)]
            return t

        def run_group(g, t):
            Vtok, Bhtok, Khtok, Aak, Arb, Ark, WT, UT = (t[k] for k in ("Vtok", "Bhtok", "Khtok", "Aak", "Arb", "Ark", "WT", "UT"))
            P, PT, Tm, Tb, Sst, Sb, PCt = t["P"], t["PT"], t["Tm"], t["Tb"], t["Sst"], t["Sb"], t["PCt"]
            Y, Ysq, mean, rstd, yo = t["Y"], t["Ysq"], t["mean"], t["rstd"], t["yo"]
            b = g // (H // GH)
            h0 = (g % (H // GH)) * GH
            rows = slice(h0 * 64, (h0 + GH) * 64)
            o.LD("sp", PCt, d["sPC"][rows, b * (S // C):(b + 1) * (S // C)].rearrange("(h j) c -> j h c", j=64))
            cur = 0
            o.MS("pool", Sst[0], 0.0)
            o.MS("pool", Sb[0], 0.0)
            nld = 0
            for sc in range(S // SCT):
                tok0 = b * S + sc * SCT
                ib = t["inb"][nld % 2]
                nld += 1
                for nm in names:
                    o.LD("sp", ib[nm], d[nm][rows, tok0:tok0 + SCT].rearrange("(h j) t -> j h t", j=64))
                for cc in range(SCT // C):
                    ci = sc * (SCT // C) + cc
                    X = {nm: ib[nm][:, :, cc * C:(cc + 1) * C] for nm in names}
                    S0, S1 = Sst[cur], Sst[1 - cur]
                    S0b, S1b = Sb[cur], Sb[1 - cur]
                    for src_, dst in ((X["sV"], Vtok), (X["sBh"], Bhtok), (X["sKh"], Khtok)):
                        ps = o.pst()
                        permm(ps, lambda h: src_[:, h, :], lambda h: id64b)
                        o.CP("act", dst, ps3(ps))
                    ps = o.pst()
                    permm(ps, lambda h: X["sBt"][:, h, :], lambda h: X["sAt"][:, h, :])
                    o.T2("dve", P[0], ps3(ps), Msu, ALU.mult)
                    ps = o.pst()
                    permm(ps, lambda h: X["sAt"][:, h, :], lambda h: X["sBt"][:, h, :])
                    o.T2("dve", PT[0], ps3(ps), Msl, ALU.mult)
                    ps = o.pst()
                    permm(ps, lambda h: X["sKt"][:, h, :], lambda h: X["sAt"][:, h, :])
                    o.T2("dve", Aak, ps3(ps), Msu, ALU.mult)
                    ps = o.pst()
                    permm(ps, lambda h: X["sBt"][:, h, :], lambda h: X["sRt"][:, h, :])
                    o.T2("dve", Arb, ps3(ps), Miu, ALU.mult)
                    ps = o.pst()
                    permm(ps, lambda h: X["sKt"][:, h, :], lambda h: X["sRt"][:, h, :])
                    o.T2("dve", Ark, ps3(ps), Miu, ALU.mult)
                    o.T2("pool", Tm[0], P[0], Meye, ALU.add)
                    o.T2("dve", Tb[0], P[0], Meye, ALU.add)
                    yield
                    pc, tcur = 0, 0
                    nlev = 6
                    for lev in range(1, nlev):
                        pn = 1 - pc
                        if lev < nlev - 1:
                            ps = o.pst()
                            permm(ps, lambda h: PT[pc][:, h, :], lambda h: P[pc][:, h, :])
                            o.CP("act", P[pn], ps3(ps))
                        ps = o.pst()
                        permm(ps, lambda h: P[pc][:, h, :], lambda h: PT[pc][:, h, :])
                        o.CP("act", PT[pn], ps3(ps))
                        pc = pn
                        yield
                        ps = o.pst()
                        permm(ps, lambda h: PT[pc][:, h, :], lambda h: Tb[tcur][:, h, :])
                        o.T2("dve", Tm[1 - tcur], ps3(ps), Tm[tcur], ALU.add)
                        o.CP("act", Tb[1 - tcur], Tm[1 - tcur])
                        tcur = 1 - tcur
                    Tf = Tb[tcur]
                    ps = o.pst()
                    for h in range(GH):
                        o.MM(ps[0:64, h * C:(h + 1) * C], X["sAt"][:, h, :], S0b[:, h, :], True, False)
                        o.MM(ps[0:64, h * C:(h + 1) * C], Aak[:, h, :], Vtok[:, h, :], False, True)
                    o.CP("act", WT, ps3(ps))
                    yield
                    ps = o.pst()
                    permm(ps, lambda h: Tf[:, h, :], lambda h: WT[:, h, :])
                    o.CP("act", UT, ps3(ps))
                    yield
                    ps = o.pst()
                    for h in range(GH):
                        sl = ps[0:64, h * C:(h + 1) * C]
                        o.MM(sl, S0b[:, h, :], X["sRt"][:, h, :], True, False)
                        o.MM(sl, UT[:, h, :], Arb[:, h, :], False, False)
                        o.MM(sl, Vtok[:, h, :], Ark[:, h, :], False, True)
                    o.CP("dve", Y, ps3(ps))
                    ps = o.pst()
                    for h in range(GH):
                        sl = ps[0:64, h * C:(h + 1) * C]
                        o.MM(sl, Bhtok[:, h, :], UT[:, h, :], True, False)
                        o.MM(sl, Khtok[:, h, :], Vtok[:, h, :], False, True)
                    pcb = PCt.v(PCt.ap[:, :, ci:ci + 1].to_broadcast([64, GH, C]))
                    o.T2("pool", S1, S0, pcb, ALU.mult)
                    o.T2("dve", S1, S1, ps3(ps), ALU.add)
                    o.CP("act", S1b, S1)
                    cur = 1 - cur
                    yield
                    o.A(Ysq, Y, AF.Square)
                    ps1 = o.pst()
                    o.MM(ps1[0:64, 0:W], ones64, flat(Y), True, True)
                    ps2 = o.pst()
                    o.MM(ps2[0:64, 0:W], ones64, flat(Ysq), True, True)
                    o.A(flat(mean), ps1[0:64, 0:W], AF.Copy, scale=1.0 / 64)
                    o.T2("pool", rstd, mean, mean, ALU.mult)
                    o.STT("dve", flat(rstd), ps2[0:64, 0:W], 1.0 / 64, flat(rstd), ALU.mult, ALU.subtract)
                    o.TS("dve", rstd, rstd, GN_EPS, None, ALU.add)
                    o.A(rstd, rstd, AF.Sqrt)
                    o.RCP(rstd, rstd)
                    o.T2("pool", yo, Y, mean, ALU.subtract)
                    o.T2("pool", yo, yo, rstd, ALU.mult)
                    gg = gng.v(gng.ap[:, h0:h0 + GH].unsqueeze(2).to_broadcast([64, GH, C]))
                    gb = gnb.v(gnb.ap[:, h0:h0 + GH].unsqueeze(2).to_broadcast([64, GH, C]))
                    o.T2("dve", yo, yo, gg, ALU.mult)
                    o.T2("dve", yo, yo, gb, ALU.add)
                    o.T2("dve", yo, yo, X["sbv"], ALU.add)
                    o.T2("pool", yo, yo, X["sg"], ALU.mult)
                    t0 = tok0 + cc * C
                    o.ST("sp", d["ybT"][rows, t0:t0 + C].rearrange("(h j) t -> j h t", j=64), yo)
                    yield

        NSLOT = min(3, NG)
        slots = [make_slot() for _ in range(NSLOT)]
        for g0 in range(0, NG, NSLOT):
            gens = [run_group(g0 + i, slots[i]) for i in range(NSLOT) if g0 + i < NG]
            live = list(gens)
            while live:
                for gn in list(live):
                    try:
                        next(gn)
                    except StopIteration:
                        live.remove(gn)
        cx.end_stage()


def stage_mixout(cx, l, xin, xout):
    cfg, d = cx.cfg, cx.dram
    o = Ops(cx)
    D, S, NB, TT, CB = cfg.D, cfg.S, cfg.NB, cfg.TT, cfg.CB
    CBB, KC = CB // 128, D // 128
    gB0 = cfg.c1 + D
    Wr = d["w_rwkv_proj"][l].rearrange("(kc p) n -> p kc n", p=128)
    Wo = d["w_out"][l].rearrange("(kc p) n -> p kc n", p=128)
    KM = max(CBB, KC)
    with ExitStack() as es:
        wb = [o.tile(es, [128, KM, 512], BF16, "wb") for _ in range(2)]
        yb = o.tile(es, [128, CBB, TT], BF16, "yb")
        mix = o.tile(es, [128, KC, TT], BF16, "mix")
        gat = [o.tile(es, [128, TT], F32, "gat") for _ in range(2)]
        ma = [o.tile(es, [128, TT], F32, "ma") for _ in range(2)]
        xt = [o.tile(es, [128, D], F32, "xt") for _ in range(TT // 128)]
        ot = [o.tile(es, [128, 512], F32, "ot") for _ in range(2)]
        bc = Bcast(cx, es)
        GT = Tl(bc.GT, bc.dt)
        nw = 0
        ntp = 0
        for b in range(NB):
            bc.build(cx, l, b, 1)
            for st in range(S // TT):
                tok0 = b * S + st * TT
                o.LD("pool", yb, d["ybT"][:, tok0:tok0 + TT].rearrange("(kc p) t -> p kc t", p=128))
                for c0 in range(0, D, 512):
                    cw = min(512, D - c0)
                    w = wb[nw % 2]
                    nw += 1
                    o.LD("pool", w[:, 0:CBB, 0:cw], Wr[:, :, c0:c0 + cw])
                    for s0 in range(0, cw, 128):
                        nb = (c0 + s0) // 128
                        s = nb % 2
                        o.LD("sp", gat[s], d["projT"][gB0 + nb * 128:gB0 + (nb + 1) * 128, tok0:tok0 + TT])
                        o.LD("sp", ma[s], d["mixAT"][nb * 128:(nb + 1) * 128, tok0:tok0 + TT])
                        o.A(gat[s], gat[s], AF.Sigmoid)
                        ps = o.pst()
                        for kc in range(CBB):
                            o.MM(ps[:, 0:TT], w[:, kc, s0:s0 + 128], yb[:, kc, :], kc == 0, kc == CBB - 1)
                        o.T2("dve", gat[s], ps[:, 0:TT], gat[s], ALU.mult)
                        o.T2("pool", mix[:, nb, :], gat[s], ma[s], ALU.add)
                NT = TT // 128
                for tt in range(NT):
                    r0 = tok0 + tt * 128
                    o.LD("sp", xt[tt], d[xin][r0:r0 + 128, :])
                for c0 in range(0, D, 512):
                    cw = min(512, D - c0)
                    w = wb[nw % 2]
                    nw += 1
                    o.LD("pool", w[:, 0:KC, 0:cw], Wo[:, :, c0:c0 + cw])
                    for tt in range(NT):
                        ps = o.pst()
                        for kc in range(KC):
                            o.MM(ps[:, 0:cw], mix[:, kc, tt * 128:(tt + 1) * 128], w[:, kc, 0:cw], kc == 0, kc == KC - 1)
                        tp = ot[ntp % 2]
                        ntp += 1
                        o.T2("dve", tp[:, 0:cw], ps[:, 0:cw], GT[:, c0:c0 + cw], ALU.mult)
                        o.T2("pool", xt[tt][:, c0:c0 + cw], xt[tt][:, c0:c0 + cw], tp[:, 0:cw], ALU.add)
                for tt in range(NT):
                    r0 = tok0 + tt * 128
                    o.ST("sp", d[xout][r0:r0 + 128, :], xt[tt])
        cx.end_stage()


I32 = mybir.dt.int32


def moe_dims(cfg):
    import os
    BLK = 256 if cfg.TOK >= 2048 else int(os.environ.get("MOE_BLK", "128"))
    NBLK = (4 * cfg.TOK) // BLK + cfg.E
    NJ = cfg.TOK // BLK + 1
    return BLK, NBLK, NJ


def moe_pieces(cfg):
    NPG = cfg.DE // 128
    DW = min(512, cfg.D)
    NPD = cfg.D // DW
    return NPG, DW, NPD


def _gather(o, out, dram2d, idx):
    oa, ia = out.ap, idx.ap
    return o.fw.dma("pool", lambda e: e.indirect_dma_start(
        out=oa, out_offset=None, in_=dram2d, in_offset=bass.IndirectOffsetOnAxis(ap=ia, axis=0)),
        reads=[idx.d], writes=[out.d])


def _scatter(o, dram2d, idx, in_):
    ia, sa = idx.ap, in_.ap
    return o.fw.dma("pool", lambda e: e.indirect_dma_start(
        out=dram2d, out_offset=bass.IndirectOffsetOnAxis(ap=ia, axis=0), in_=sa, in_offset=None),
        reads=[idx.d, in_.d])


def stage_route(cx, l, xin):
    cfg, d = cx.cfg, cx.dram
    o = Ops(cx)
    D, S, NB, E, TOK, DE = cfg.D, cfg.S, cfg.NB, cfg.E, cfg.TOK, cfg.DE
    KC, FC = D // 128, DE // 128
    NTL = TOK // 128
    BLK, NBLK, NJ = moe_dims(cfg)
    with ExitStack() as es:
        K = o.consts(es)
        lstrict = o.tile(es, [128, 128], F32, "lstrict")
        o.LD("sp", lstrict, d["c_lstrict"])
        rw = o.tile(es, [128, KC, E], F32, "rw")
        o.LD("sp", rw, d["router_w"][l].rearrange("(kc p) e -> p kc e", p=128))
        rb = o.tile(es, [1, E], F32, "rb")
        o.LD("sp", rb, d["router_b"][l])
        xt = [o.tile(es, [128, D], F32, "xt") for _ in range(2)]
        ut = Tl(cx.sb(es, [128, D], F32, "ut"))
        junk = o.tile(es, [128, D], F32, "junk")
        small = o.tile(es, [128, 4], F32, "small")
        hT32 = o.tile(es, [128, KC, 128], F32, "hT32")
        lg = o.tile(es, [128, NTL, E], F32, "lg")
        t8 = o.tile(es, [128, NTL, 8], F32, "t8")
        mask = o.tile(es, [128, E], F32, "mask")
        rank = o.tile(es, [128, NTL, E], F32, "rank")
        carry = o.tile(es, [128, E], F32, "carry")
        o.MS("dve", carry, 0.0)
        bc = Bcast(cx, es)
        i = 0
        for b in range(NB):
            bc.build(cx, l, b, 2)
            for tl in range(S // 128):
                s = i % 2
                r0 = i * 128
                o.LD("sp", xt[s], d[xin][r0:r0 + 128, :])
                norm_mod_tile(cx, xt[s].ap, xt[s].d, ut.ap, ut.d, junk.ap, junk.d, small.ap, small.d,
                              bc.GS, bc.dg, bc.SH, bc.ds, D)
                o.ST("sp", d["Hd"][r0:r0 + 128, :], ut)
                transpose_to_fm(cx, {"ident": K["ident"].ap, "dep": K["ident"].d}, ut.ap, ut.d, KC,
                                [(hT32.ap, hT32.d, "act")])
                ps = o.pst()
                for kc in range(KC):
                    o.MM(ps[:, 0:E], hT32[:, kc, :], rw[:, kc, :], kc == 0, False)
                o.MM(ps[:, 0:E], K["ones128"][0:1, :], rb, False, True)
                o.CP("dve", lg[:, i, :], ps[:, 0:E])
                t8a, lga = t8.ap[:, i, :], lg.ap[:, i, :]
                cx.fw.op("dve", lambda e, t8a=t8a, lga=lga: e.max(out=t8a, in_=lga), reads=[lg.d], writes=[t8.d])
                o.TS("dve", mask, lg[:, i, :], t8[:, i, 3:4], None, ALU.is_ge)
                ps2 = o.pst()
                o.MM(ps2[:, 0:E], lstrict, mask, True, True)
                ps3 = o.pst()
                o.MM(ps3[:, 0:E], K["ones128"], mask, True, True)
                o.T2("dve", rank[:, i, :], ps2[:, 0:E], carry, ALU.add)
                o.T2("dve", carry, carry, ps3[:, 0:E], ALU.add)
                i += 1
        thr = o.tile(es, [128, E, NJ], F32, "thr")
        o.LD("sp", thr, d["c_thr"])
        cmp3 = o.tile(es, [128, E, NJ], F32, "cmp3")
        o.T2("dve", cmp3, carry.v(carry.ap.unsqueeze(2).to_broadcast([128, E, NJ])), thr, ALU.is_gt)
        nblk = o.tile(es, [128, E], F32, "nblk")
        ca, na = cmp3.ap, nblk.ap
        cx.fw.op("dve", lambda e: e.reduce_sum(out=na, in_=ca, axis=AX.X), reads=[cmp3.d], writes=[nblk.d])
        onesE = o.tile(es, [128, E], F32, "onesE")
        o.MS("dve", onesE, 1.0)
        incl = o.tile(es, [128, E], F32, "incl")
        ia_, oa_ = incl.ap, onesE.ap
        cx.fw.op("dve", lambda e: e.tensor_tensor_scan(out=ia_, data0=oa_, data1=na, initial=0.0, op0=ALU.mult, op1=ALU.add),
                 reads=[nblk.d, onesE.d], writes=[incl.d])
        pstart = o.tile(es, [128, E], F32, "pstart")
        pend = o.tile(es, [128, E], F32, "pend")
        o.T2("dve", pstart, incl, nblk, ALU.subtract)
        o.TS("dve", pstart, pstart, float(BLK), None, ALU.mult)
        o.TS("dve", pend, incl, float(BLK), None, ALU.mult)
        bst = o.tile(es, [128, NBLK, E], F32, "bst")
        o.LD("sp", bst, d["c_blkstart"])
        o.T2("dve", bst, pend.v(pend.ap.unsqueeze(1).to_broadcast([128, NBLK, E])), bst, ALU.is_le)
        be = o.tile(es, [128, NBLK], F32, "be")
        ba, bsa = be.ap, bst.ap
        cx.fw.op("dve", lambda e: e.reduce_sum(out=ba, in_=bsa, axis=AX.X), reads=[bst.d], writes=[be.d])
        o.TS("dve", be, be, float(E - 1), None, ALU.min)
        NPG, DW, NPD = moe_pieces(cfg)
        for nm, n2, mul, offc in (("idx_gu", NPG, 128.0 * NPG, "c_guoff"), ("idx_d", NPD, 128.0 * NPD, "c_doff"),
                                  ("idx_b", 2, None, "c_boff")):
            off = o.tile(es, [128, n2], F32, "off_" + nm)
            o.LD("sp", off, d[offc])
            tf = o.tile(es, [128, NBLK, n2], F32, "tf_" + nm)
            ti = o.tile(es, [128, NBLK, n2], I32, "ti_" + nm)
            beb = be.v(be.ap.unsqueeze(2).to_broadcast([128, NBLK, n2]))
            offb = off.v(off.ap.unsqueeze(1).to_broadcast([128, NBLK, n2]))
            if mul is not None:
                o.STT("dve", tf, beb, mul, offb, ALU.mult, ALU.add)
            else:
                mcol = o.tile(es, [128, 2], F32, "mcol")
                o.LD("sp", mcol, d["c_bmul"])
                o.T2("dve", tf, beb, mcol.v(mcol.ap.unsqueeze(1).to_broadcast([128, NBLK, n2])), ALU.mult)
                o.T2("dve", tf, tf, offb, ALU.add)
            o.CP("dve", ti, tf)
            o.ST("sp", d[nm], ti)
        w4 = o.tile(es, [128, NTL, 4], F32, "w4")
        o.T2("dve", w4, t8[:, :, 0:4], t8.v(t8.ap[:, :, 0:1].to_broadcast([128, NTL, 4])), ALU.subtract)
        o.A(w4, w4, AF.Exp)
        ssum = o.tile(es, [128, NTL], F32, "ssum")
        wa, sa = w4.ap, ssum.ap
        cx.fw.op("dve", lambda e: e.reduce_sum(out=sa, in_=wa, axis=AX.X), reads=[w4.d], writes=[ssum.d])
        o.RCP(ssum, ssum)
        o.T2("dve", w4, w4, ssum.v(ssum.ap.unsqueeze(2).to_broadcast([128, NTL, 4])), ALU.mult)
        o.ST("sp", d["w4"], w4)
        o.T2("dve", rank, rank, pstart.v(pstart.ap.unsqueeze(1).to_broadcast([128, NTL, E])), ALU.add)
        sel = o.tile(es, [128, NTL, E], F32, "sel")
        d4f = o.tile(es, [128, NTL, 4], F32, "d4f")
        d4 = o.tile(es, [128, NTL, 4], I32, "d4")
        for k in range(4):
            o.T2("dve", sel, lg, t8.v(t8.ap[:, :, k:k + 1].to_broadcast([128, NTL, E])), ALU.is_equal)
            o.T2("dve", sel, sel, rank, ALU.mult)
            sla, dka = sel.ap, d4f.ap[:, :, k]
            cx.fw.op("dve", lambda e, sla=sla, dka=dka: e.reduce_sum(out=dka, in_=sla, axis=AX.X),
                     reads=[sel.d], writes=[d4f.d])
        o.CP("dve", d4, d4f)
        o.ST("sp", d["d4"], d4)
        for i in range(NTL):
            s = i % 2
            o.LD("sp", xt[s], d["Hd"][i * 128:(i + 1) * 128, :])
            for k in range(4):
                _scatter(o, d["Hsorted"], d4[:, i, k:k + 1], xt[s])
        cx.end_stage()


def stage_experts(cx, l):
    cfg, d = cx.cfg, cx.dram
    o = Ops(cx)
    D, E, DE = cfg.D, cfg.E, cfg.DE
    KC, FC = D // 128, DE // 128
    BLK, NBLK, NJ = moe_dims(cfg)
    NT = BLK // 128
    NPG, DW, NPD = moe_pieces(cfg)
    wgu, wd = d["w_gu3_%d" % l], d["w_d3_%d" % l]
    bgu2, bd2 = d["b_gu2_%d" % l], d["b_d2_%d" % l]
    GW = KC * 256
    DWW = FC * DW
    SW = max(GW, DWW)
    with ExitStack() as es:
        K = o.consts(es)
        igu = o.tile(es, [128, NBLK, NPG], I32, "igu")
        o.LD("sp", igu, d["idx_gu"])
        idd = o.tile(es, [128, NBLK, NPD], I32, "idd")
        o.LD("sp", idd, d["idx_d"])
        ib = o.tile(es, [128, NBLK, 2], I32, "ib")
        o.LD("sp", ib, d["idx_b"])
        stgw = [o.tile(es, [128, SW], F32, "stgw") for _ in range(2)]
        wg = [o.tile(es, [128, KC, 256], BF16, "wg") for _ in range(3)]
        wdn = [o.tile(es, [128, FC, DW], BF16, "wdn") for _ in range(3)]
        bg = [o.tile(es, [128, 2 * FC], F32, "bg") for _ in range(2)]
        bdb = [o.tile(es, [128, D], F32, "bdb") for _ in range(2)]
        ht = [Tl(cx.sb(es, [128, D], F32, "ht")) for _ in range(NT)]
        hTb = o.tile(es, [128, KC, BLK], BF16, "hTb")
        actg = o.tile(es, [128, FC, BLK], BF16, "actg")
        gc = [o.tile(es, [128, BLK], F32, "gc") for _ in range(2)]
        sg = [o.tile(es, [128, BLK], F32, "sg") for _ in range(2)]
        uc = [o.tile(es, [128, BLK], F32, "uc") for _ in range(2)]
        yst = [o.tile(es, [128, DW], F32, "yst") for _ in range(4)]
        cnt = {"stg": 0, "wg": 0, "wd": 0, "act": 0, "yst": 0, "cast": 0}

        def fetch(kind, blk, piece):
            st = stgw[cnt["stg"] % 2]
            cnt["stg"] += 1
            if kind == "g":
                dst = wg[cnt["wg"] % 3]
                cnt["wg"] += 1
                _gather(o, st[:, 0:GW], wgu, igu[:, blk, piece:piece + 1])
                src = st.v(st.ap[:, 0:GW].rearrange("p (k n) -> p k n", k=KC))
            else:
                dst = wdn[cnt["wd"] % 3]
                cnt["wd"] += 1
                _gather(o, st[:, 0:DWW], wd, idd[:, blk, piece:piece + 1])
                src = st.v(st.ap[:, 0:DWW].rearrange("p (k n) -> p k n", k=FC))
            nk = KC if kind == "g" else FC
            h = nk // 2
            o.CP("act", dst[:, 0:h, :], src[:, 0:h, :])
            o.CP("dve", dst[:, h:nk, :], src[:, h:nk, :])
            return dst

        order = [(blk, kind, pc) for blk in range(NBLK) for kind, n in (("g", NPG), ("d", NPD)) for pc in range(n)]
        fetched = {}

        def ensure(k):
            if k < len(order) and k not in fetched:
                fetched[k] = fetch(order[k][1], order[k][0], order[k][2])

        ensure(0)
        k = 0
        for tt in range(NT):
            o.LD("sp", ht[tt], d["Hsorted"][tt * 128:(tt + 1) * 128, :])
        for blk in range(NBLK):
            s = blk % 2
            _gather(o, bg[s], bgu2, ib[:, blk, 0:1])
            _gather(o, bdb[s], bd2, ib[:, blk, 1:2])
            for tt in range(NT):
                transpose_to_fm(cx, {"ident": K["ident"].ap, "dep": K["ident"].d}, ht[tt].ap, ht[tt].d, KC,
                                [(hTb.ap[:, :, tt * 128:(tt + 1) * 128], hTb.d, "alt")])
                if blk + 1 < NBLK:
                    r1 = (blk + 1) * BLK + tt * 128
                    o.LD("sp", ht[tt], d["Hsorted"][r1:r1 + 128, :])
            for fidx in range(NPG):
                w = fetched.pop(k)
                ensure(k + 1)
                k += 1
                psg = o.pst()
                for kc in range(KC):
                    o.MM(psg[:, 0:BLK], w[:, kc, 0:128], hTb[:, kc, :], kc == 0, kc == KC - 1)
                psu = o.pst()
                for kc in range(KC):
                    o.MM(psu[:, 0:BLK], w[:, kc, 128:256], hTb[:, kc, :], kc == 0, kc == KC - 1)
                a = cnt["act"] % 2
                cnt["act"] += 1
                o.TS("dve", gc[a], psg[:, 0:BLK], bg[s][:, fidx:fidx + 1], SW_LIMIT, ALU.add, ALU.min)
                o.A(sg[a], gc[a], AF.Sigmoid, scale=SW_ALPHA)
                o.TS("dve", uc[a], psu[:, 0:BLK], bg[s][:, FC + fidx:FC + fidx + 1], SW_LIMIT, ALU.add, ALU.min)
                o.TS("dve", uc[a], uc[a], -SW_LIMIT, 1.0, ALU.max, ALU.add)
                o.T2("dve", gc[a], gc[a], sg[a], ALU.mult)
                o.T2("dve", actg[:, fidx, :], gc[a], uc[a], ALU.mult)
            for pc in range(NPD):
                w = fetched.pop(k)
                ensure(k + 1)
                k += 1
                for tt in range(NT):
                    ps = o.pst()
                    for fc in range(FC):
                        o.MM(ps[:, 0:DW], actg[:, fc, tt * 128:(tt + 1) * 128], w[:, fc, :], fc == 0, fc == FC - 1)
                    col = pc * DW
                    y_ = yst[cnt["yst"] % 4]
                    cnt["yst"] += 1
                    o.T2("dve", y_, ps[:, 0:DW], bdb[s][:, col:col + DW], ALU.add)
                    r0 = blk * BLK + tt * 128
                    o.ST("sp", d["Ybuf"][r0:r0 + 128, col:col + DW], y_)
        cx.end_stage()


def stage_combine(cx, l, xin, xout, final=False):
    cfg, d = cx.cfg, cx.dram
    o = Ops(cx)
    D, S, NB, TOK = cfg.D, cfg.S, cfg.NB, cfg.TOK
    NTL = TOK // 128
    with ExitStack() as es:
        d4 = o.tile(es, [128, NTL, 4], I32, "d4")
        o.LD("sp", d4, d["d4"])
        w4 = o.tile(es, [128, NTL, 4], F32, "w4")
        o.LD("sp", w4, d["w4"])
        yk = [o.tile(es, [128, D], F32, "yk") for _ in range(4)]
        xt = [o.tile(es, [128, D], F32, "xt") for _ in range(2)]
        acc = [o.tile(es, [128, D], F32, "acc") for _ in range(2)]
        bc = Bcast(cx, es)
        GT = Tl(bc.GT, bc.dt)
        if final:
            fg = o.tile(es, [128, D], F32, "fg")
            o.LD("sp", fg, d["final_g"][0:1, :].partition_broadcast(128))
            junk = o.tile(es, [128, D], F32, "junk")
            small = o.tile(es, [128, 4], F32, "small")
        i = 0
        for b in range(NB):
            bc.build(cx, l, b, 2)
            for tl in range(S // 128):
                s = i % 2
                r0 = i * 128
                o.LD("sp", xt[s], d[xin][r0:r0 + 128, :])
                for k in range(4):
                    _gather(o, yk[k], d["Ybuf"], d4[:, i, k:k + 1])
                a = acc[s]
                o.TS("dve", a, yk[0], w4[:, i, 0:1], None, ALU.mult)
                for k in range(1, 4):
                    o.STT("dve", a, yk[k], w4[:, i, k:k + 1], a, ALU.mult, ALU.add)
                o.T2("dve", a, a, GT, ALU.mult)
                o.T2("dve", a, a, xt[s], ALU.add)
                if final:
                    norm_mod_tile(cx, a.ap, a.d, xt[s].ap, xt[s].d, junk.ap, junk.d, small.ap, small.d,
                                  fg.ap, fg.d, None, None, D)
                    o.ST("sp", d["out"][r0:r0 + 128, :], xt[s])
                else:
                    o.ST("sp", d[xout][r0:r0 + 128, :], a)
                i += 1
        cx.end_stage()


def all_stages(cfg):
    st = []
    for l in range(cfg.L):
        st.append(lambda cx, l=l: stage_mod(cx, l))
    xin = "x"
    for l in range(cfg.L):
        xm = "x%d" % (2 * l + 1)
        xo = "x%d" % (2 * l + 2)
        last = l == cfg.L - 1
        st.append(lambda cx, l=l, xin=xin: stage_inproj(cx, l, xin))
        st.append(lambda cx, l=l: stage_conv(cx, l))
        st.append(lambda cx, l=l: stage_rwkv_prep(cx, l))
        st.append(lambda cx, l=l: stage_rwkv_scan(cx, l))
        st.append(lambda cx, l=l, xin=xin, xm=xm: stage_mixout(cx, l, xin, xm))
        st.append(lambda cx, l=l, xm=xm: stage_route(cx, l, xm))
        st.append(lambda cx, l=l: stage_experts(cx, l))
        st.append(lambda cx, l=l, xm=xm, xo=xo, last=last: stage_combine(cx, l, xm, xo, final=last))
        xin = xo
    return st


def shared_prep(cfg, inp):
    m = host_prep(cfg, inp, 0)
    m.pop("x")
    m.pop("cT")
    return m


def core_prep(cfg, inp, core):
    c = cfg
    KC = c.D // 128
    b0 = core * c.NB
    x = np.ascontiguousarray(inp["x"][b0:b0 + c.NB].reshape(c.NB * c.S, c.D), dtype=np.float32)
    cT = np.ascontiguousarray(inp["c"][b0:b0 + c.NB].T.reshape(KC, 128, c.NB).transpose(1, 0, 2), dtype=np.float32)
    return {"x": x, "cT": cT}


def kernel(**inputs):
    cfg = Cfg()
    inp = {k: np.asarray(v) for k, v in inputs.items()}
    nc = build_program(cfg, all_stages(cfg), outputs=("out",))
    shared = shared_prep(cfg, inp)
    in_maps = []
    for core in range(cfg.NCORES):
        m = dict(shared)
        m.update(core_prep(cfg, inp, core))
        in_maps.append(m)
    res = run_bass_kernel_spmd(nc, in_maps, core_ids=list(range(cfg.NCORES)))
    B = cfg.NB * cfg.NCORES
    out = np.concatenate([np.asarray(r["out"], dtype=np.float32).reshape(cfg.NB, cfg.S, cfg.D) for r in res.results], axis=0)
    return out.reshape(B, cfg.S, cfg.D)
```

```python
import numpy as np
from contextlib import ExitStack
import concourse.bass as bass
import concourse.mybir as mybir
from concourse.bass_utils import run_bass_kernel_spmd

F32 = mybir.dt.float32
BF16 = mybir.dt.bfloat16
AF = mybir.ActivationFunctionType
ALU = mybir.AluOpType
AX = mybir.AxisListType

ENGS = ("pe", "act", "dve", "pool", "sp")
RMS_EPS = 1e-5
LN_EPS = 1e-5
GN_EPS = 64e-5
SW_LIMIT = 7.0
SW_ALPHA = 1.702
EXP_M05 = float(np.exp(-0.5))


class Cfg:
    def __init__(self, D=2048, S=2048, NB=2, CA=1024, H=32, E=32, DE=1024, L=2, NCORES=8):
        self.D, self.S, self.NB, self.CA, self.H, self.E, self.DE, self.L = D, S, NB, CA, H, E, DE, L
        self.NCORES = NCORES
        self.N = 64
        self.CB = H * 64
        self.RD, self.RA, self.RG, self.RMV, self.KW = 96, 96, 256, 64, 31
        self.RW = 3 * self.CB + self.RD + self.RA + self.RG
        self.INC = 2 * CA + self.RW + 2 * D
        self.INCX = self.INC + self.RMV
        self.TOK = NB * S
        self.TT = min(512, S)
        self.C = 64
        self.c0 = 2 * CA
        self.c1 = self.c0 + self.RW


class Dep:
    __slots__ = ("w", "r")

    def __init__(self):
        self.w = None
        self.r = {}


class FW:
    def __init__(self, nc, n_dma_sems=32):
        self.nc = nc
        self.stream = {e: [] for e in ENGS}
        self.sems = {}
        self.cnt = {}
        self.seen = {e: {} for e in ENGS}
        for e in ENGS:
            self.sems["E" + e] = nc.alloc_semaphore("sem_" + e)
            self.cnt["E" + e] = 0
        self.dma_keys = []
        for i in range(n_dma_sems):
            k = "D%d" % i
            self.sems[k] = nc.alloc_semaphore("semd%d" % i)
            self.cnt[k] = 0
            self.dma_keys.append(k)
        self.dma_rr = 0

    def _need(self, eng, reads, writes):
        seen = self.seen[eng]
        st = self.stream[eng]
        if eng == "pe":
            seen["Epe"] = self.cnt["Epe"]
        for d in reads:
            if d.w is not None and seen.get(d.w[0], 0) < d.w[1]:
                seen[d.w[0]] = d.w[1]
                st.append(("wait", d.w[0], d.w[1]))
        for d in writes:
            if d.w is not None and seen.get(d.w[0], 0) < d.w[1]:
                seen[d.w[0]] = d.w[1]
                st.append(("wait", d.w[0], d.w[1]))
            for k, v in d.r.items():
                if seen.get(k, 0) < v:
                    seen[k] = v
                    st.append(("wait", k, v))

    def _mark(self, ev, reads, writes):
        k, v = ev
        for d in reads:
            if d.r.get(k, 0) < v:
                d.r[k] = v
        for d in writes:
            d.w = ev
            d.r = {}

    def op(self, eng, fn, reads=(), writes=()):
        self._need(eng, reads, writes)
        k = "E" + eng
        self.cnt[k] += 1
        ev = (k, self.cnt[k])
        self.stream[eng].append(("op", fn, k, 1))
        self._mark(ev, reads, writes)
        return ev

    def dma(self, q, fn, reads=(), writes=()):
        k = self.dma_keys[self.dma_rr % len(self.dma_keys)]
        self.dma_rr += 1
        if self.cnt[k] > 0 and self.seen[q].get(k, 0) < self.cnt[k]:
            self.seen[q][k] = self.cnt[k]
            self.stream[q].append(("wait", k, self.cnt[k]))
        self._need(q, reads, writes)
        self.cnt[k] += 16
        ev = (k, self.cnt[k])
        self.stream[q].append(("op", fn, k, 16))
        self._mark(ev, reads, writes)
        return ev

    def barrier(self):
        for e in ENGS:
            for k, v in self.cnt.items():
                if v > 0 and self.seen[e].get(k, 0) < v:
                    self.seen[e][k] = v
                    self.stream[e].append(("wait", k, v))

    def emit(self):
        nc = self.nc
        fw = self
        names = {"pe": "tensor", "act": "scalar", "dve": "vector", "pool": "gpsimd", "sp": "sync"}
        with nc.Block() as block:
            for e in ENGS:
                items = self.stream[e]

                def body(eng, items=items):
                    for it in items:
                        if it[0] == "wait":
                            eng.wait_ge(fw.sems[it[1]], it[2])
                        else:
                            it[1](eng).then_inc(fw.sems[it[2]], it[3])
                getattr(block, names[e])(body)
        self.stream = {e: [] for e in ENGS}


class Ctx:
    def __init__(self, nc, cfg):
        self.nc = nc
        self.cfg = cfg
        self.fw = FW(nc)
        self.ps = [nc.alloc_psum_tensor("psb%d" % i, [128, 512], F32).ap() for i in range(8)]
        self.psd = [Dep() for _ in range(8)]
        self.ps_rr = 0
        self.uid = 0
        self.dram = {}

    def psum(self):
        i = self.ps_rr % 8
        self.ps_rr += 1
        return self.ps[i], self.psd[i]

    def name(self, p):
        self.uid += 1
        return "%s_%d" % (p, self.uid)

    def sb(self, es, shape, dt=F32, tag="t"):
        t = es.enter_context(self.nc.sbuf_tensor(self.name(tag), list(shape), dt))
        return t.ap() if hasattr(t, "ap") else t

    def dma(self, q, out, in_, reads=(), writes=(), **kw):
        return self.fw.dma(q, lambda e: e.dma_start(out=out, in_=in_, **kw), reads=reads, writes=writes)

    def act(self, out, in_, func, reads=(), writes=(), eng="act", **kw):
        return self.fw.op("act", lambda e: e.activation(out=out, in_=in_, func=func, **kw), reads=reads, writes=writes)

    def tt(self, eng, out, in0, in1, op, reads=(), writes=()):
        return self.fw.op(eng, lambda e: e.tensor_tensor(out=out, in0=in0, in1=in1, op=op), reads=reads, writes=writes)

    def ts(self, eng, out, in0, s1, s2, op0, op1=None, reads=(), writes=()):
        if op1 is None:
            return self.fw.op(eng, lambda e: e.tensor_scalar(out=out, in0=in0, scalar1=s1, scalar2=None, op0=op0),
                              reads=reads, writes=writes)
        return self.fw.op(eng, lambda e: e.tensor_scalar(out=out, in0=in0, scalar1=s1, scalar2=s2, op0=op0, op1=op1),
                          reads=reads, writes=writes)

    def stt(self, eng, out, in0, scalar, in1, op0, op1, reads=(), writes=()):
        return self.fw.op(eng, lambda e: e.scalar_tensor_tensor(out=out, in0=in0, scalar=scalar, in1=in1, op0=op0, op1=op1),
                          reads=reads, writes=writes)

    def copy(self, eng, out, in_, reads=(), writes=()):
        if eng == "act":
            return self.fw.op("act", lambda e: e.activation(out=out, in_=in_, func=AF.Copy), reads=reads, writes=writes)
        return self.fw.op(eng, lambda e: e.tensor_copy(out=out, in_=in_), reads=reads, writes=writes)

    def mm(self, out, lhsT, rhs, start, stop, reads=(), writes=()):
        return self.fw.op("pe", lambda e: e.matmul(out, lhsT=lhsT, rhs=rhs, start=start, stop=stop),
                          reads=reads, writes=writes)

    def tr(self, out, in_, ident, reads=(), writes=()):
        return self.fw.op("pe", lambda e: e.transpose(out=out, in_=in_, identity=ident), reads=reads, writes=writes)

    def memset(self, eng, ap, val, writes=()):
        return self.fw.op(eng, lambda e: e.memset(ap, val), writes=writes)

    def end_stage(self):
        self.fw.barrier()
        import os
        if os.environ.get("KSTAT"):
            tot = {e: (sum(1 for i in v if i[0] == "op"), sum(1 for i in v if i[0] == "wait")) for e, v in self.fw.stream.items()}
            print("STAGE", tot, flush=True)
        self.fw.emit()


class Tl:
    __slots__ = ("ap", "d")

    def __init__(self, ap, d=None):
        self.ap = ap
        self.d = d if d is not None else Dep()

    def __getitem__(self, idx):
        return Tl(self.ap[idx], self.d)

    def v(self, ap):
        return Tl(ap, self.d)


def _rd(*xs):
    return [x.d for x in xs if isinstance(x, Tl)]


def _ap(x):
    return x.ap if isinstance(x, Tl) else x


class Ops:
    def __init__(self, cx):
        self.cx = cx
        self.fw = cx.fw

    def tile(self, es, shape, dt=F32, tag="t"):
        return Tl(self.cx.sb(es, shape, dt, tag))

    def pst(self):
        ap, d = self.cx.psum()
        return Tl(ap, d)

    def A(self, out, in_, func, scale=None, bias=None, accum=None):
        kw = {}
        if scale is not None:
            kw["scale"] = _ap(scale)
        if bias is not None:
            kw["bias"] = _ap(bias)
        if accum is not None:
            kw["accum_out"] = accum.ap
        o, i = out.ap, in_.ap
        wr = [out.d] + ([accum.d] if accum is not None else [])
        return self.fw.op("act", lambda e: e.activation(out=o, in_=i, func=func, **kw),
                          reads=_rd(in_, scale, bias), writes=wr)

    def T2(self, eng, out, a, b, op):
        o, x, y = out.ap, a.ap, b.ap
        return self.fw.op(eng, lambda e: e.tensor_tensor(out=o, in0=x, in1=y, op=op), reads=_rd(a, b), writes=[out.d])

    def TS(self, eng, out, a, s1, s2, op0, op1=None):
        o, x, p1, p2 = out.ap, a.ap, _ap(s1), _ap(s2)
        if op1 is None:
            return self.fw.op(eng, lambda e: e.tensor_scalar(out=o, in0=x, scalar1=p1, scalar2=None, op0=op0),
                              reads=_rd(a, s1), writes=[out.d])
        return self.fw.op(eng, lambda e: e.tensor_scalar(out=o, in0=x, scalar1=p1, scalar2=p2, op0=op0, op1=op1),
                          reads=_rd(a, s1, s2), writes=[out.d])

    def STT(self, eng, out, a, scalar, b, op0, op1):
        o, x, sc, y = out.ap, a.ap, _ap(scalar), b.ap
        eng = "dve"
        return self.fw.op(eng, lambda e: e.scalar_tensor_tensor(out=o, in0=x, scalar=sc, in1=y, op0=op0, op1=op1),
                          reads=_rd(a, scalar, b), writes=[out.d])

    def CP(self, eng, out, in_):
        o, i = out.ap, in_.ap
        if eng == "act":
            return self.fw.op("act", lambda e: e.activation(out=o, in_=i, func=AF.Copy), reads=[in_.d], writes=[out.d])
        return self.fw.op(eng, lambda e: e.tensor_copy(out=o, in_=i), reads=[in_.d], writes=[out.d])

    def RCP(self, out, in_):
        o, i = out.ap, in_.ap
        return self.fw.op("dve", lambda e: e.reciprocal(out=o, in_=i), reads=[in_.d], writes=[out.d])

    def MS(self, eng, out, val):
        o = out.ap
        return self.fw.op(eng, lambda e: e.memset(o, val), writes=[out.d])

    def MM(self, out, lhsT, rhs, start, stop):
        o, l, r = out.ap, lhsT.ap, rhs.ap
        return self.fw.op("pe", lambda e: e.matmul(o, lhsT=l, rhs=r, start=start, stop=stop),
                          reads=[lhsT.d, rhs.d], writes=[out.d])

    def TR(self, out, in_, ident):
        o, i, idn = out.ap, in_.ap, ident.ap
        return self.fw.op("pe", lambda e: e.transpose(out=o, in_=i, identity=idn), reads=[in_.d, ident.d], writes=[out.d])

    def LD(self, q, out, dram_ap):
        o = out.ap
        return self.fw.dma(q, lambda e: e.dma_start(out=o, in_=dram_ap), writes=[out.d])

    def ST(self, q, dram_ap, in_):
        i = in_.ap
        return self.fw.dma(q, lambda e: e.dma_start(out=dram_ap, in_=i), reads=[in_.d])

    def consts(self, es):
        d = self.cx.dram
        K = {}
        for nm in ("ident", "ones128", "bdones"):
            K[nm] = self.tile(es, [128, 128], F32, nm)
            self.LD("sp", K[nm], d["c_" + nm])
        return K


def load_consts(cx, es):
    d = cx.dram
    K = {}
    dep = Dep()
    for nm, shape in (("ident", [128, 128]), ("ones128", [128, 128]), ("bdones", [128, 128])):
        K[nm] = cx.sb(es, shape, F32, nm)
        cx.dma("sp", K[nm], d["c_" + nm], writes=[dep])
    K["dep"] = dep
    return K


def stage_mod(cx, l):
    cfg, d = cx.cfg, cx.dram
    D, NB = cfg.D, cfg.NB
    KC = D // 128
    CBW = 256
    with ExitStack() as es:
        cT = cx.sb(es, [128, KC, NB], F32, "cT")
        ones = cx.sb(es, [1, 128], F32, "ones1")
        brow = [cx.sb(es, [1, CBW], F32, "brow") for _ in range(2)]
        wb = [cx.sb(es, [128, KC, CBW], F32, "wmod") for _ in range(2)]
        out = cx.sb(es, [NB, 6 * D], F32, "modsb")
        dc, do, dout = Dep(), Dep(), Dep()
        dbr = [Dep(), Dep()]
        dwb = [Dep(), Dep()]
        cx.dma("sp", cT, d["cT"], writes=[dc])
        cx.act(cT, cT, AF.Silu, reads=[dc], writes=[dc])
        cx.memset("dve", ones, 1.0, writes=[do])
        nblk = 6 * D // CBW
        wview = d["w_mod"][l].rearrange("(kc p) n -> p kc n", p=128)
        for cb in range(nblk):
            s = cb % 2
            cx.dma("sp", wb[s], wview[:, :, cb * CBW:(cb + 1) * CBW], writes=[dwb[s]])
            cx.dma("sp", brow[s], d["b_mod"][l:l + 1, cb * CBW:(cb + 1) * CBW], writes=[dbr[s]])
            ps, pd = cx.psum()
            for kc in range(KC):
                cx.mm(ps[0:NB, 0:CBW], cT[:, kc, :], wb[s][:, kc, :], kc == 0, False,
                      reads=[dc, dwb[s]], writes=[pd])
            cx.mm(ps[0:NB, 0:CBW], ones[0:1, 0:NB], brow[s], False, True, reads=[do, dbr[s]], writes=[pd])
            cx.copy("dve", out[:, cb * CBW:(cb + 1) * CBW], ps[0:NB, 0:CBW], reads=[pd], writes=[dout])
        cx.dma("sp", d["mod%d" % l], out, reads=[dout])
        cx.end_stage()


class Bcast:
    def __init__(self, cx, es):
        D = cx.cfg.D
        self.GS = cx.sb(es, [128, D], F32, "GS")
        self.SH = cx.sb(es, [128, D], F32, "SH")
        self.GT = cx.sb(es, [128, D], F32, "GT")
        self.tmp = cx.sb(es, [128, D], F32, "gtmp")
        self.dg, self.ds, self.dt, self.dtmp = Dep(), Dep(), Dep(), Dep()

    def build(self, cx, l, b, which):
        cfg, d = cx.cfg, cx.dram
        D = cfg.D
        off = 0 if which == 1 else 3 * D
        mod = d["mod%d" % l]
        gname = "norm1_g" if which == 1 else "norm2_g"
        cx.dma("sp", self.SH, mod[b:b + 1, off:off + D].partition_broadcast(128), writes=[self.ds])
        cx.dma("sp", self.GS, mod[b:b + 1, off + D:off + 2 * D].partition_broadcast(128), writes=[self.dg])
        cx.dma("sp", self.GT, mod[b:b + 1, off + 2 * D:off + 3 * D].partition_broadcast(128), writes=[self.dt])
        cx.dma("sp", self.tmp, d[gname][l:l + 1, :].partition_broadcast(128), writes=[self.dtmp])
        cx.stt("dve", self.GS, self.GS, 1.0, self.tmp, ALU.add, ALU.mult, reads=[self.dtmp], writes=[self.dg])


def norm_mod_tile(cx, xt, dx, ut, du, junk, dj, small, dsm, GS, dgs, SH, dsh, D):
    ss, sq, rs = small[:, 0:1], small[:, 1:2], small[:, 2:3]
    cx.fw.op("act", lambda e: e.activation(out=junk, in_=xt, func=AF.Square, accum_out=ss),
             reads=[dx], writes=[dj, dsm])
    cx.ts("dve", sq, ss, 1.0 / D, RMS_EPS, ALU.mult, ALU.add, reads=[dsm], writes=[dsm])
    cx.act(sq, sq, AF.Sqrt, reads=[dsm], writes=[dsm])
    cx.fw.op("dve", lambda e: e.reciprocal(out=rs, in_=sq), reads=[dsm], writes=[dsm])
    cx.stt("dve", ut, xt, rs, GS, ALU.mult, ALU.mult, reads=[dx, dsm, dgs], writes=[du])
    if SH is not None:
        cx.tt("pool", ut, ut, SH, ALU.add, reads=[dsh], writes=[du])


def transpose_to_fm(cx, K, ut, du, KC, dsts):
    for k0 in range(0, KC, 4):
        n = min(4, KC - k0)
        ps, pd = cx.psum()
        for j in range(n):
            cx.tr(ps[:, j * 128:(j + 1) * 128], ut[:, (k0 + j) * 128:(k0 + j + 1) * 128], K["ident"],
                  reads=[du, K["dep"]], writes=[pd])
        src = ps[:, 0:n * 128].rearrange("p (a b) -> p a b", a=n)
        for (dst, dd, eng) in dsts:
            if eng == "alt":
                eng = "act" if (k0 // 4) % 2 == 0 else "dve"
            cx.copy(eng, dst[:, k0:k0 + n, :], src, reads=[pd], writes=[dd])


def stage_inproj(cx, l, xin):
    cfg, d = cx.cfg, cx.dram
    D, S, NB, TT = cfg.D, cfg.S, cfg.NB, cfg.TT
    KC = D // 128
    NCOL = cfg.INCX
    W = d["w_inx"][l].rearrange("(kc p) n -> p kc n", p=128)
    projT = d["projT"]
    x = d[xin]
    with ExitStack() as es:
        K = load_consts(cx, es)
        xt = [cx.sb(es, [128, D], F32, "xt") for _ in range(2)]
        dxt = [Dep(), Dep()]
        ut = cx.sb(es, [128, D], F32, "ut")
        du = Dep()
        junk = cx.sb(es, [128, D], F32, "junk")
        dj = Dep()
        small = cx.sb(es, [128, 4], F32, "small")
        dsm = Dep()
        uT = cx.sb(es, [128, KC, TT], BF16, "uT")
        duT = Dep()
        wb = [cx.sb(es, [128, KC, 512], BF16, "wb") for _ in range(2)]
        dwb = [Dep(), Dep()]
        stg = [cx.sb(es, [128, TT], F32, "stg") for _ in range(4)]
        dstg = [Dep() for _ in range(4)]
        nst = 0
        nw = 0
        bc = Bcast(cx, es)
        for b in range(NB):
            if True:
                bc.build(cx, l, b, 1)
                GS, dgs, SH, dsh = bc.GS, bc.dg, bc.SH, bc.ds
                for st in range(S // TT):
                    tok0 = b * S + st * TT
                    for tt in range(TT // 128):
                        s = tt % 2
                        r0 = tok0 + tt * 128
                        cx.dma("sp", xt[s], x[r0:r0 + 128, :], writes=[dxt[s]])
                        norm_mod_tile(cx, xt[s], dxt[s], ut, du, junk, dj, small, dsm, GS, dgs, SH, dsh, D)
                        transpose_to_fm(cx, K, ut, du, KC,
                                        [(uT[:, :, tt * 128:(tt + 1) * 128], duT, "act" if tt % 2 else "dve")])
                    for c0 in range(0, NCOL, 512):
                        cw = min(512, NCOL - c0)
                        s = nw % 2
                        nw += 1
                        cx.dma("pool", wb[s][:, :, 0:cw], W[:, :, c0:c0 + cw], writes=[dwb[s]])
                        for s0 in range(0, cw, 128):
                            nsz = min(128, cw - s0)
                            ps, pd = cx.psum()
                            for kc in range(KC):
                                cx.mm(ps[0:nsz, 0:TT], wb[s][:, kc, s0:s0 + nsz], uT[:, kc, :], kc == 0, kc == KC - 1,
                                      reads=[dwb[s], duT], writes=[pd])
                            q = nst % 4
                            nst += 1
                            cx.copy("act" if q % 2 else "dve", stg[q][0:nsz, :], ps[0:nsz, 0:TT], reads=[pd], writes=[dstg[q]])
                            cx.dma("sp", projT[c0 + s0:c0 + s0 + nsz, tok0:tok0 + TT], stg[q][0:nsz, :], reads=[dstg[q]])
        cx.end_stage()


def dram_specs(cfg):
    c = cfg
    L, D, CA, CB, E, DE, NB, TOK = c.L, c.D, c.CA, c.CB, c.E, c.DE, c.NB, c.TOK
    KC, CAB, CBB = D // 128, CA // 128, CB // 128
    BLK, NBLK, NJ = moe_dims(cfg)
    NPG, DW, NPD = moe_pieces(cfg)
    NTL = TOK // 128
    ins = [
        ("x", [TOK, D]), ("cT", [128, KC, NB]),
        ("norm1_g", [L, D]), ("norm2_g", [L, D]), ("final_g", [1, D]),
        ("w_mod", [L, D, 6 * D]), ("b_mod", [L, 6 * D]),
        ("w_inx", [L, D, c.INCX]),
        ("conv_wT", [L, 128, CAB, c.KW]), ("conv_b", [L, 128, CAB]), ("conv_ln_g", [L, 128, CAB]),
        ("conv_ln_b", [L, 128, CAB]), ("w_conv_proj", [L, CA, D]),
        ("mu_rkv", [L, 128, 3 * CBB]), ("mu_w", [L, c.RD, 1]), ("mu_a", [L, c.RA, 1]), ("mu_g", [L, 128, c.RG // 128]),
        ("w0", [L, 128, CBB]), ("a0", [L, 128, CBB]), ("k_k", [L, 128, CBB]), ("k_a", [L, 128, CBB]),
        ("r_k", [L, 128, CBB]), ("gn_g", [L, 64, c.H]), ("gn_b", [L, 64, c.H]),
        ("w2", [L, c.RD, CB]), ("a2", [L, c.RA, CB]), ("g2", [L, c.RG, CB]),
        ("v0", [L, 128, CBB]), ("mu_v", [L, c.RMV, 1]), ("v2", [L, c.RMV, CB]),
        ("w_rwkv_proj", [L, CB, D]), ("w_out", [L, D, D]),
        ("router_w", [L, D, E]), ("router_b", [L, 1, E]),
        ("c_ident", [128, 128]), ("c_ones128", [128, 128]), ("c_bdones", [128, 128]),
        ("c_masks", [64, 4, 8, 64]), ("c_reset", [128, 512]),
        ("c_lstrict", [128, 128]), ("c_thr", [128, E, NJ]), ("c_blkstart", [128, NBLK, E]),
        ("c_guoff", [128, NPG]), ("c_doff", [128, NPD]), ("c_boff", [128, 2]), ("c_bmul", [128, 2]),
    ]
    for l in range(L):
        ins += [("w_gu3_%d" % l, [E * 128 * NPG, KC * 256]), ("w_d3_%d" % l, [E * 128 * NPD, (DE // 128) * DW]),
                ("b_gu2_%d" % l, [E * 128, 2 * DE // 128]), ("b_d2_%d" % l, [E, D])]
    scr = [("mod%d" % l, [NB, 6 * D], F32) for l in range(L)]
    scr += [("projT", [c.INCX, TOK], F32), ("mixAT", [D, TOK], F32)]
    scr += [(n, [CB, TOK], BF16) for n in ("sAt", "sRt", "sBt", "sKt", "sBh", "sKh", "sV")]
    scr += [(n, [CB, TOK], F32) for n in ("sbv", "sg", "vfirstT", "ybT")]
    scr += [("sPC", [CB, TOK // 64], F32)]
    scr += [("x%d" % i, [TOK, D], F32) for i in range(1, 2 * L + 1)]
    scr += [("out", [TOK, D], F32), ("Hd", [TOK, D], F32), ("Hsorted", [NBLK * BLK, D], F32),
            ("Ybuf", [NBLK * BLK, D], F32), ("idx_gu", [128, NBLK, NPG], I32),
            ("idx_d", [128, NBLK, NPD], I32), ("idx_b", [128, NBLK, 2], I32),
            ("w4", [128, NTL, 4], F32), ("d4", [128, NTL, 4], I32)]
    return [(n, s, F32, "in") for n, s in ins] + [(n, s, dt, "scratch") for n, s, dt in scr]


def build_program(cfg, stages, outputs=("out",), ext_inputs=()):
    nc = bass.Bass("TRN2", target_bir_lowering=False)
    cx = Ctx(nc, cfg)
    for n, s, dt, role in dram_specs(cfg):
        if role == "in" or n in ext_inputs:
            kind = "ExternalInput"
        elif n in outputs:
            kind = "ExternalOutput"
        else:
            kind = "Internal"
        cx.dram[n] = nc.dram_tensor(n, list(s), dt, kind=kind).ap()
    for st in stages:
        st(cx)
    return nc


def host_prep(cfg, inp, core):
    c = cfg
    L, D, CA, CB, NB = c.L, c.D, c.CA, c.CB, c.NB
    KC, CAB, CBB = D // 128, CA // 128, CB // 128
    f = lambda a: np.ascontiguousarray(a, dtype=np.float32)
    b0 = core * NB
    m = {}
    m["x"] = f(inp["x"][b0:b0 + NB].reshape(NB * c.S, D))
    m["cT"] = f(inp["c"][b0:b0 + NB].T.reshape(KC, 128, NB).transpose(1, 0, 2))
    for k in ("norm1_g", "norm2_g", "w_mod", "b_mod", "w_conv_proj", "w2", "a2", "g2", "w_rwkv_proj", "w_out",
              "router_w"):
        m[k] = f(inp[k])
    m["final_g"] = f(inp["final_g"].reshape(1, D))
    v1p = np.zeros((L, D, c.RMV), np.float32)
    v1p[1:] = inp["v1"]
    m["w_inx"] = f(np.concatenate([inp["w_in"], v1p], axis=2))
    m["conv_wT"] = f(inp["conv_w"][:, :, 0, :].transpose(0, 2, 1).reshape(L, CAB, 128, c.KW).transpose(0, 2, 1, 3))
    pc = lambda a, nb: f(a.reshape(L, nb, 128).transpose(0, 2, 1))
    for k in ("conv_b", "conv_ln_g", "conv_ln_b"):
        m[k] = pc(inp[k], CAB)
    mu = inp["mu_shift"]
    m["mu_rkv"] = f(mu[:, :3 * CB].reshape(L, 3 * CBB, 128).transpose(0, 2, 1))
    o = 3 * CB
    m["mu_w"] = f(mu[:, o:o + c.RD].reshape(L, c.RD, 1))
    m["mu_a"] = f(mu[:, o + c.RD:o + c.RD + c.RA].reshape(L, c.RA, 1))
    m["mu_g"] = f(mu[:, o + c.RD + c.RA:].reshape(L, c.RG // 128, 128).transpose(0, 2, 1))
    for k in ("w0", "a0", "k_k", "k_a", "r_k"):
        m[k] = pc(inp[k], CBB)
    for k in ("gn_g", "gn_b"):
        m[k] = f(inp[k].reshape(L, c.H, 64).transpose(0, 2, 1))
    z = lambda a: np.concatenate([np.zeros((1,) + a.shape[1:], np.float32), a], axis=0)
    m["v0"] = pc(z(inp["v0"]), CBB)
    m["mu_v"] = f(z(inp["mu_v"]).reshape(L, c.RMV, 1))
    m["v2"] = f(z(inp["v2"]))
    m["router_b"] = f(inp["router_b"].reshape(L, 1, c.E))
    E, DE = c.E, c.DE
    HW = DE // 2
    NPG, DW, NPD = moe_pieces(c)
    FC = DE // 128
    for l in range(L):
        w = inp["w_gate_up"][l]
        g_ = w[..., :DE].reshape(E, KC, 128, NPG, 128)
        u_ = w[..., DE:].reshape(E, KC, 128, NPG, 128)
        gu_ = np.stack([g_, u_], axis=4)
        m["w_gu3_%d" % l] = f(gu_.transpose(0, 2, 3, 1, 4, 5).reshape(E * 128 * NPG, KC * 256))
        wd_ = inp["w_down"][l].reshape(E, FC, 128, NPD, DW)
        m["w_d3_%d" % l] = f(wd_.transpose(0, 2, 3, 1, 4).reshape(E * 128 * NPD, FC * DW))
        m["b_gu2_%d" % l] = f(inp["b_gate_up"][l].reshape(E, 2 * DE // 128, 128).transpose(0, 2, 1).reshape(E * 128, 2 * DE // 128))
        m["b_d2_%d" % l] = f(inp["b_down"][l])
    BLK, NBLK, NJ = moe_dims(c)
    pp = np.arange(128)
    m["c_lstrict"] = (pp[:, None] < pp[None, :]).astype(np.float32)
    m["c_thr"] = f(np.broadcast_to((np.arange(NJ) * BLK)[None, None, :], (128, E, NJ)))
    m["c_blkstart"] = f(np.broadcast_to((np.arange(NBLK) * BLK)[None, :, None], (128, NBLK, E)))
    m["c_guoff"] = f(pp[:, None] * NPG + np.arange(NPG)[None, :])
    m["c_doff"] = f(pp[:, None] * NPD + np.arange(NPD)[None, :])
    m["c_boff"] = f(np.stack([pp, np.zeros(128)], axis=1))
    m["c_bmul"] = f(np.stack([np.full(128, 128.0), np.ones(128)], axis=1))
    m["c_ident"] = np.eye(128, dtype=np.float32)
    m["c_ones128"] = np.ones((128, 128), np.float32)
    bd = np.zeros((128, 128), np.float32)
    bd[:64, :64] = 1
    bd[64:, 64:] = 1
    m["c_bdones"] = bd
    s_ = np.arange(64)[:, None]
    t_ = np.arange(64)[None, :]
    masks = np.stack([(s_ < t_), (s_ > t_), (s_ <= t_), (s_ == t_)]).astype(np.float32)
    m["c_masks"] = f(np.broadcast_to(masks[:, :, None, :], (4, 64, 8, 64)).transpose(1, 0, 2, 3))
    rs = np.ones((128, 512), np.float32)
    rs[:, ::64] = 0
    m["c_reset"] = rs
    return m


def stage_conv(cx, l):
    cfg, d = cx.cfg, cx.dram
    D, S, NB, TT, CA, KW = cfg.D, cfg.S, cfg.NB, cfg.TT, cfg.CA, cfg.KW
    CAB = CA // 128
    HL = KW - 1
    projT = d["projT"]
    gA0 = cfg.c1
    with ExitStack() as es:
        K = load_consts(cx, es)
        cw = cx.sb(es, [128, CAB, KW], F32, "cw")
        cb_ = cx.sb(es, [128, CAB], F32, "cb")
        lg = cx.sb(es, [128, CAB], F32, "lg")
        lb = cx.sb(es, [128, CAB], F32, "lb")
        dpar = Dep()
        cx.dma("sp", cw, d["conv_wT"][l], writes=[dpar])
        cx.dma("sp", cb_, d["conv_b"][l], writes=[dpar])
        cx.dma("sp", lg, d["conv_ln_g"][l], writes=[dpar])
        cx.dma("sp", lb, d["conv_ln_b"][l], writes=[dpar])
        Wp = cx.sb(es, [128, CAB, D], BF16, "Wp")
        dWp = Dep()
        wv = d["w_conv_proj"][l].rearrange("(kc p) n -> p kc n", p=128)
        for kc in range(CAB):
            cx.dma("pool", Wp[:, kc, :], wv[:, kc, :], writes=[dWp])
        a_t = [cx.sb(es, [128, HL + TT], F32, "a_t") for _ in range(2)]
        g_t = [cx.sb(es, [128, HL + TT], F32, "g_t") for _ in range(2)]
        da = [Dep(), Dep()]
        dg = [Dep(), Dep()]
        accs = [cx.sb(es, [128, CAB, TT], F32, "acc") for _ in range(2)]
        sqs = [cx.sb(es, [128, CAB, TT], F32, "sq") for _ in range(2)]
        daccs = [[Dep() for _ in range(CAB)] for _ in range(2)]
        dsqs = [[Dep() for _ in range(CAB)] for _ in range(2)]
        ntile = 0
        mean = cx.sb(es, [128, TT], F32, "mean")
        rstd = cx.sb(es, [128, TT], F32, "rstd")
        dmean, drstd = Dep(), Dep()
        cT = cx.sb(es, [128, CAB, TT], BF16, "cT")
        dcT = Dep()
        gat = [cx.sb(es, [128, TT], F32, "gat") for _ in range(2)]
        dgat = [Dep(), Dep()]
        stg = [cx.sb(es, [128, TT], F32, "stg") for _ in range(2)]
        dstg = [Dep(), Dep()]
        n = 0
        for b in range(NB):
            for st in range(S // TT):
                t0 = st * TT
                tok0 = b * S + t0
                acc, sq, dacc, dsq = accs[ntile % 2], sqs[ntile % 2], daccs[ntile % 2], dsqs[ntile % 2]
                ntile += 1
                for cb in range(CAB):
                    s = n % 2
                    n += 1
                    if t0 == 0:
                        cx.memset("pool", a_t[s][:, 0:HL], 0.0, writes=[da[s]])
                        cx.memset("pool", g_t[s][:, 0:HL], 0.0, writes=[dg[s]])
                        cx.dma("sp", a_t[s][:, HL:], projT[cb * 128:(cb + 1) * 128, tok0:tok0 + TT], writes=[da[s]])
                        cx.dma("sp", g_t[s][:, HL:], projT[CA + cb * 128:CA + (cb + 1) * 128, tok0:tok0 + TT], writes=[dg[s]])
                    else:
                        cx.dma("sp", a_t[s], projT[cb * 128:(cb + 1) * 128, tok0 - HL:tok0 + TT], writes=[da[s]])
                        cx.dma("sp", g_t[s], projT[CA + cb * 128:CA + (cb + 1) * 128, tok0 - HL:tok0 + TT], writes=[dg[s]])
                    cx.act(g_t[s], g_t[s], AF.Sigmoid, reads=[dg[s]], writes=[dg[s]])
                    cx.tt("pool", a_t[s], a_t[s], g_t[s], ALU.mult, reads=[dg[s]], writes=[da[s]])
                    o = acc[:, cb, :]
                    cx.ts("dve", o, a_t[s][:, 0:TT], cw[:, cb, 0:1], cb_[:, cb:cb + 1], ALU.mult, ALU.add,
                          reads=[da[s], dpar], writes=[dacc[cb]])
                    for k in range(1, KW):
                        cx.stt("dve", o, a_t[s][:, k:k + TT], cw[:, cb, k:k + 1], o, ALU.mult, ALU.add,
                               reads=[da[s]], writes=[dacc[cb]])
                    cx.act(sq[:, cb, :], o, AF.Square, reads=[dacc[cb]], writes=[dsq[cb]])
                ps1, pd1 = cx.psum()
                for cb in range(CAB):
                    cx.mm(ps1[:, 0:TT], K["ones128"], acc[:, cb, :], cb == 0, cb == CAB - 1,
                          reads=[K["dep"], dacc[cb]], writes=[pd1])
                ps2, pd2 = cx.psum()
                for cb in range(CAB):
                    cx.mm(ps2[:, 0:TT], K["ones128"], sq[:, cb, :], cb == 0, cb == CAB - 1,
                          reads=[dsq[cb]], writes=[pd2])
                cx.fw.op("act", lambda e, ps1=ps1: e.mul(out=mean, in_=ps1[:, 0:TT], mul=1.0 / CA), reads=[pd1], writes=[dmean])
                cx.tt("dve", rstd, mean, mean, ALU.mult, reads=[dmean], writes=[drstd])
                cx.stt("dve", rstd, ps2[:, 0:TT], 1.0 / CA, rstd, ALU.mult, ALU.subtract, reads=[pd2], writes=[drstd])
                cx.ts("dve", rstd, rstd, LN_EPS, None, ALU.add, reads=[], writes=[drstd])
                cx.act(rstd, rstd, AF.Sqrt, reads=[drstd], writes=[drstd])
                cx.fw.op("dve", lambda e: e.reciprocal(out=rstd, in_=rstd), reads=[drstd], writes=[drstd])
                for cb in range(CAB):
                    o = acc[:, cb, :]
                    eng = "dve" if cb % 2 == 0 else "pool"
                    cx.tt(eng, o, o, mean, ALU.subtract, reads=[dmean], writes=[dacc[cb]])
                    cx.tt(eng, o, o, rstd, ALU.mult, reads=[drstd], writes=[dacc[cb]])
                    cx.act(cT[:, cb, :], o, AF.Silu, reads=[dacc[cb], dpar], writes=[dcT],
                           scale=lg[:, cb:cb + 1], bias=lb[:, cb:cb + 1])
                for nb in range(D // 128):
                    s = nb % 2
                    cx.dma("sp", gat[s], projT[gA0 + nb * 128:gA0 + (nb + 1) * 128, tok0:tok0 + TT], writes=[dgat[s]])
                    cx.act(gat[s], gat[s], AF.Sigmoid, reads=[dgat[s]], writes=[dgat[s]])
                    ps, pd = cx.psum()
                    for kc in range(CAB):
                        cx.mm(ps[:, 0:TT], Wp[:, kc, nb * 128:(nb + 1) * 128], cT[:, kc, :], kc == 0, kc == CAB - 1,
                              reads=[dWp, dcT], writes=[pd])
                    cx.tt("dve", stg[s], ps[:, 0:TT], gat[s], ALU.mult, reads=[pd, dgat[s]], writes=[dstg[s]])
                    cx.dma("sp", d["mixAT"][nb * 128:(nb + 1) * 128, tok0:tok0 + TT], stg[s], reads=[dstg[s]])
        cx.end_stage()


def stage_rwkv_prep(cx, l):
    cfg, d = cx.cfg, cx.dram
    o = Ops(cx)
    D, S, NB, TT, CB = cfg.D, cfg.S, cfg.NB, cfg.TT, cfg.CB
    CBB = CB // 128
    C = cfg.C
    NCH = TT // C
    RD, RA, RG, RMV = cfg.RD, cfg.RA, cfg.RG, cfg.RMV
    projT = d["projT"]
    c0 = cfg.c0
    with ExitStack() as es:
        K = o.consts(es)
        reset = o.tile(es, [128, TT], F32, "reset")
        o.LD("sp", reset, d["c_reset"][:, 0:TT])
        mu_rkv = o.tile(es, [128, 3 * CBB], F32, "mu_rkv")
        o.LD("sp", mu_rkv, d["mu_rkv"][l])
        mu_w = o.tile(es, [RD, 1], F32, "mu_w")
        o.LD("sp", mu_w, d["mu_w"][l])
        mu_a = o.tile(es, [RA, 1], F32, "mu_a")
        o.LD("sp", mu_a, d["mu_a"][l])
        mu_g = o.tile(es, [128, RG // 128], F32, "mu_g")
        o.LD("sp", mu_g, d["mu_g"][l])
        mu_v = o.tile(es, [RMV, 1], F32, "mu_v")
        o.LD("sp", mu_v, d["mu_v"][l])
        vecs = {}
        for nm in ("w0", "a0", "k_k", "k_a", "r_k", "v0"):
            vecs[nm] = o.tile(es, [128, CBB], F32, nm)
            o.LD("sp", vecs[nm], d[nm][l])
        omka = o.tile(es, [128, CBB], F32, "omka")
        o.TS("dve", omka, vecs["k_a"], -1.0, 1.0, ALU.mult, ALU.add)
        w2 = o.tile(es, [RD, CB], F32, "w2")
        o.LD("sp", w2, d["w2"][l])
        a2 = o.tile(es, [RA, CB], F32, "a2")
        o.LD("sp", a2, d["a2"][l])
        g2 = o.tile(es, [128, RG // 128, CB], F32, "g2")
        o.LD("sp", g2, d["g2"][l].rearrange("(j p) n -> p j n", p=128))
        v2 = o.tile(es, [RMV, CB], F32, "v2")
        o.LD("sp", v2, d["v2"][l])

        def T(tag, rows=128, cols=TT):
            return o.tile(es, [rows, cols], F32, tag)

        raw = [T("raw%d" % i, 128, TT + 1) for i in range(6)]
        nraw = [0]

        def shifted(out, row0, rows, tok0, first, mu, eng):
            rw = raw[nraw[0] % 6][0:rows]
            nraw[0] += 1
            if first:
                o.MS("pool", rw[:, 0:1], 0.0)
                o.LD("sp", rw[:, 1:], projT[row0:row0 + rows, tok0:tok0 + TT])
            else:
                o.LD("sp", rw, projT[row0:row0 + rows, tok0 - 1:tok0 + TT])
            o.T2(eng, out, rw[:, 0:TT], rw[:, 1:TT + 1], ALU.subtract)
            o.STT(eng, out, out, mu, rw[:, 1:TT + 1], ALU.mult, ALU.add)

        tw, zaT, zvT = T("tw", RD), T("zaT", RA), T("zvT", RMV)
        sgz = o.tile(es, [128, RG // 128, TT], F32, "sgz")

        def make_set():
            W_ = {nm: T(nm) for nm in ("r", "k", "v", "lw", "a", "kk", "tmp", "tmp2", "keff", "bs",
                                       "G", "Gx", "eG", "eGx", "enG", "eH", "vf")}
            W_["PC"] = o.tile(es, [128, NCH], F32, "PC")
            outs_ = {nm: o.tile(es, [128, TT], BF16, "o_" + nm) for nm in ("sAt", "sRt", "sBt", "sKt", "sBh", "sKh", "sV")}
            outs_.update({nm: T("o_" + nm) for nm in ("sbv", "sg")})
            W_["outs"] = outs_
            return W_

        wsets = [make_set(), make_set()]
        for b in range(NB):
            for st in range(S // TT):
                tok0 = b * S + st * TT
                first = st == 0
                ro = c0 + 3 * CB
                shifted(tw, ro, RD, tok0, first, mu_w[:, 0:1], "dve")
                o.A(tw, tw, AF.Tanh)
                shifted(zaT, ro + RD, RA, tok0, first, mu_a[:, 0:1], "pool")
                for j in range(RG // 128):
                    shifted(sgz[:, j, :], ro + RD + RA + j * 128, 128, tok0, first, mu_g[:, j:j + 1], "dve")
                o.A(sgz, sgz, AF.Sigmoid)
                if l > 0:
                    shifted(zvT, cfg.INC, RMV, tok0, first, mu_v[:, 0:1], "pool")
                def prep_cb(cb, W_):
                    cs = slice(cb * 128, (cb + 1) * 128)
                    col = lambda t, j=cb: t[:, j:j + 1]
                    r, k, v, lw, a, kk, tmp, tmp2, keff, bs = (W_[n_] for n_ in ("r", "k", "v", "lw", "a", "kk", "tmp", "tmp2", "keff", "bs"))
                    G, Gx, eG, eGx, enG, eH, vf, PC, outs = (W_[n_] for n_ in ("G", "Gx", "eG", "eGx", "enG", "eH", "vf", "PC", "outs"))
                    shifted(r, c0 + cb * 128, 128, tok0, first, mu_rkv[:, cb:cb + 1], "dve")
                    shifted(k, c0 + CB + cb * 128, 128, tok0, first, mu_rkv[:, CBB + cb:CBB + cb + 1], "pool")
                    shifted(v, c0 + 2 * CB + cb * 128, 128, tok0, first, mu_rkv[:, 2 * CBB + cb:2 * CBB + cb + 1], "dve")
                    ps = o.pst()
                    o.MM(ps[:, 0:TT], w2[:, cs], tw, True, True)
                    o.A(lw, ps[:, 0:TT], AF.Sigmoid, bias=col(vecs["w0"]))
                    o.TS("pool", lw, lw, -EXP_M05, None, ALU.mult)
                    yield
                    ps = o.pst()
                    o.MM(ps[:, 0:TT], a2[:, cs], zaT, True, True)
                    o.A(a, ps[:, 0:TT], AF.Sigmoid, bias=col(vecs["a0"]))
                    yield
                    ps = o.pst()
                    nj = RG // 128
                    for j in range(nj):
                        o.MM(ps[:, 0:TT], g2[:, j, cs], sgz[:, j, :], j == 0, j == nj - 1)
                    o.CP("act", outs["sg"], ps[:, 0:TT])
                    o.ST("sp", d["sg"][cs, tok0:tok0 + TT], outs["sg"])
                    yield
                    if l == 0:
                        o.ST("sp", d["vfirstT"][cs, tok0:tok0 + TT], v)
                    else:
                        ps = o.pst()
                        o.MM(ps[:, 0:TT], v2[:, cs], zvT, True, True)
                        o.A(tmp, ps[:, 0:TT], AF.Sigmoid, bias=col(vecs["v0"]))
                        o.LD("sp", vf, d["vfirstT"][cs, tok0:tok0 + TT])
                        o.T2("pool", vf, vf, v, ALU.subtract)
                        o.T2("pool", vf, vf, tmp, ALU.mult)
                        o.T2("pool", v, v, vf, ALU.add)
                    o.CP("act", outs["sV"], v)
                    o.ST("sp", d["sV"][cs, tok0:tok0 + TT], outs["sV"])
                    yield
                    o.TS("dve", kk, k, col(vecs["k_k"]), None, ALU.mult)
                    o.T2("pool", tmp, kk, kk, ALU.mult)
                    ps = o.pst()
                    o.MM(ps[:, 0:TT], K["bdones"], tmp, True, True)
                    o.A(tmp2, ps[:, 0:TT], AF.Sqrt)
                    o.TS("dve", tmp2, tmp2, 1e-12, None, ALU.max)
                    o.RCP(tmp2, tmp2)
                    o.T2("dve", kk, kk, tmp2, ALU.mult)
                    yield
                    o.TS("pool", tmp, a, col(vecs["k_a"]), col(omka), ALU.mult, ALU.add)
                    o.T2("pool", keff, k, tmp, ALU.mult)
                    o.T2("dve", bs, kk, a, ALU.mult)
                    o.STT("dve", tmp, r, col(vecs["r_k"]), keff, ALU.mult, ALU.mult)
                    ps = o.pst()
                    o.MM(ps[:, 0:TT], K["bdones"], tmp, True, True)
                    o.T2("dve", outs["sbv"], ps[:, 0:TT], v, ALU.mult)
                    o.ST("sp", d["sbv"][cs, tok0:tok0 + TT], outs["sbv"])
                    yield
                    Ga, lwa, rsa = G.ap, lw.ap, reset.ap
                    cx.fw.op("dve", lambda e, Ga=Ga, lwa=lwa, rsa=rsa: e.tensor_tensor_scan(
                        out=Ga, data0=rsa, data1=lwa, initial=0.0, op0=ALU.mult, op1=ALU.add),
                        reads=[lw.d, reset.d], writes=[G.d])
                    o.T2("pool", Gx, G, lw, ALU.subtract)
                    o.A(eG, G, AF.Exp)
                    o.A(eGx, Gx, AF.Exp)
                    o.A(enG, G, AF.Exp, scale=-1.0)
                    yield
                    G3 = G.ap.rearrange("p (c t) -> p c t", t=C)
                    GCb = G.v(G3[:, :, C - 1:C].to_broadcast([128, NCH, C]))
                    o.T2("dve", tmp.v(tmp.ap.rearrange("p (c t) -> p c t", t=C)), GCb, G.v(G3), ALU.subtract)
                    o.A(eH, tmp, AF.Exp)
                    o.A(PC.v(PC.ap.rearrange("p (c o) -> p c o", o=1)), G.v(G3[:, :, C - 1:C]), AF.Exp)
                    ch0 = tok0 // C
                    o.ST("sp", d["sPC"][cs, ch0:ch0 + NCH], PC)
                    yield
                    o.STT("dve", outs["sAt"], kk, -1.0, eGx, ALU.mult, ALU.mult)
                    o.T2("pool", outs["sRt"], r, eG, ALU.mult)
                    o.T2("dve", outs["sBt"], bs, enG, ALU.mult)
                    o.T2("pool", outs["sKt"], keff, enG, ALU.mult)
                    o.T2("dve", outs["sBh"], bs, eH, ALU.mult)
                    o.T2("pool", outs["sKh"], keff, eH, ALU.mult)
                    for nm in ("sAt", "sRt", "sBt", "sKt", "sBh", "sKh"):
                        o.ST("sp", d[nm][cs, tok0:tok0 + TT], outs[nm])

                for cb0 in range(0, CBB, 2):
                    gens = [prep_cb(cb0 + i, wsets[i]) for i in range(2) if cb0 + i < CBB]
                    live = list(gens)
                    while live:
                        for gn in list(live):
                            try:
                                next(gn)
                            except StopIteration:
                                live.remove(gn)
        cx.end_stage()


def stage_rwkv_scan(cx, l):
    cfg, d = cx.cfg, cx.dram
    o = Ops(cx)
    S, NB, CB, H = cfg.S, cfg.NB, cfg.CB, cfg.H
    C = cfg.C
    GH = min(8, H)
    NG = NB * H // GH
    SCT = 2 * C
    W = GH * C
    names_b = ("sAt", "sRt", "sBt", "sKt", "sBh", "sKh", "sV")
    names_f = ("sbv", "sg")
    names = names_b + names_f
    with ExitStack() as es:
        K = o.consts(es)
        ones64 = K["ones128"][0:64, 0:64]
        id64b = o.tile(es, [64, 64], BF16, "id64b")
        o.CP("dve", id64b, K["ident"][0:64, 0:64])
        masks = o.tile(es, [64, 4, GH, C], F32, "masks")
        o.LD("sp", masks, d["c_masks"][:, :, 0:GH, :])
        Msu, Msl, Miu, Meye = (masks[:, i] for i in range(4))
        gng = o.tile(es, [64, H], F32, "gng")
        gnb = o.tile(es, [64, H], F32, "gnb")
        o.LD("sp", gng, d["gn_g"][l])
        o.LD("sp", gnb, d["gn_b"][l])
        def flat(t):
            return t.v(t.ap.rearrange("p h c -> p (h c)"))

        def ps3(ps):
            return ps.v(ps.ap[0:64, 0:W].rearrange("p (h c) -> p h c", h=GH))

        def permm(outps, lhs_fn, rhs_fn, first=True, last=True):
            for h in range(GH):
                o.MM(outps[0:64, h * C:(h + 1) * C], lhs_fn(h), rhs_fn(h), first, last)

        def G3(tag, dt=F32):
            return o.tile(es, [64, GH, C], dt, tag)

        def make_slot():
            t = {}
            t["inb"] = [{nm: o.tile(es, [64, GH, SCT], BF16 if nm in names_b else F32, "in_" + nm) for nm in names}
                        for _ in range(2)]
            t["PCt"] = o.tile(es, [64, GH, S // C], F32, "PCt")
            for nm in ("Vtok", "Bhtok", "Khtok", "Aak", "Arb", "Ark", "WT", "UT"):
                t[nm] = G3(nm, BF16)
            t["P"] = [G3("P0", BF16), G3("P1", BF16)]
            t["PT"] = [G3("PT0", BF16), G3("PT1", BF16)]
            t["Tm"] = [G3("T0"), G3("T1")]
            t["Tb"] = [G3("Tb0", BF16), G3("Tb1", BF16)]
            for nm in ("Y", "Ysq", "mean", "rstd", "yo"):
                t[nm] = G3(nm)
            t["Sst"] = [G3("S0"), G3("S1")]
            t["Sb"] = [G3("Sb0", BF16), G3("Sb1", BF16)]
            return t

        def run_group(g, t):
            Vtok, Bhtok, Khtok, Aak, Arb, Ark, WT, UT = (t[k] for k in ("Vtok", "Bhtok", "Khtok", "Aak", "Arb", "Ark", "WT", "UT"))
            P, PT, Tm, Tb, Sst, Sb, PCt = t["P"], t["PT"], t["Tm"], t["Tb"], t["Sst"], t["Sb"], t["PCt"]
            Y, Ysq, mean, rstd, yo = t["Y"], t["Ysq"], t["mean"], t["rstd"], t["yo"]
            b = g // (H // GH)
            h0 = (g % (H // GH)) * GH
            rows = slice(h0 * 64, (h0 + GH) * 64)
            o.LD("sp", PCt, d["sPC"][rows, b * (S // C):(b + 1) * (S // C)].rearrange("(h j) c -> j h c", j=64))
            cur = 0
            o.MS("pool", Sst[0], 0.0)
            o.MS("pool", Sb[0], 0.0)
            nld = 0
            for sc in range(S // SCT):
                tok0 = b * S + sc * SCT
                ib = t["inb"][nld % 2]
                nld += 1
                for nm in names:
                    o.LD("sp", ib[nm], d[nm][rows, tok0:tok0 + SCT].rearrange("(h j) t -> j h t", j=64))
                for cc in range(SCT // C):
                    ci = sc * (SCT // C) + cc
                    X = {nm: ib[nm][:, :, cc * C:(cc + 1) * C] for nm in names}
                    S0, S1 = Sst[cur], Sst[1 - cur]
                    S0b, S1b = Sb[cur], Sb[1 - cur]
                    for src_, dst in ((X["sV"], Vtok), (X["sBh"], Bhtok), (X["sKh"], Khtok)):
                        ps = o.pst()
                        permm(ps, lambda h: src_[:, h, :], lambda h: id64b)
                        o.CP("act", dst, ps3(ps))
                    ps = o.pst()
                    permm(ps, lambda h: X["sBt"][:, h, :], lambda h: X["sAt"][:, h, :])
                    o.T2("dve", P[0], ps3(ps), Msu, ALU.mult)
                    ps = o.pst()
                    permm(ps, lambda h: X["sAt"][:, h, :], lambda h: X["sBt"][:, h, :])
                    o.T2("dve", PT[0], ps3(ps), Msl, ALU.mult)
                    ps = o.pst()
                    permm(ps, lambda h: X["sKt"][:, h, :], lambda h: X["sAt"][:, h, :])
                    o.T2("dve", Aak, ps3(ps), Msu, ALU.mult)
                    ps = o.pst()
                    permm(ps, lambda h: X["sBt"][:, h, :], lambda h: X["sRt"][:, h, :])
                    o.T2("dve", Arb, ps3(ps), Miu, ALU.mult)
                    ps = o.pst()
                    permm(ps, lambda h: X["sKt"][:, h, :], lambda h: X["sRt"][:, h, :])
                    o.T2("dve", Ark, ps3(ps), Miu, ALU.mult)
                    o.T2("pool", Tm[0], P[0], Meye, ALU.add)
                    o.T2("dve", Tb[0], P[0], Meye, ALU.add)
                    yield
                    pc, tcur = 0, 0
                    nlev = 6
                    for lev in range(1, nlev):
                        pn = 1 - pc
                        if lev < nlev - 1:
                            ps = o.pst()
                            permm(ps, lambda h: PT[pc][:, h, :], lambda h: P[pc][:, h, :])
                            o.CP("act", P[pn], ps3(ps))
                        ps = o.pst()
                        permm(ps, lambda h: P[pc][:, h, :], lambda h: PT[pc][:, h, :])
                        o.CP("act", PT[pn], ps3(ps))
                        pc = pn
                        yield
                        ps = o.pst()
                        permm(ps, lambda h: PT[pc][:, h, :], lambda h: Tb[tcur][:, h, :])
                        o.T2("dve", Tm[1 - tcur], ps3(ps), Tm[tcur], ALU.add)
                        o.CP("act", Tb[1 - tcur], Tm[1 - tcur])
                        tcur = 1 - tcur
                    Tf = Tb[tcur]
                    ps = o.pst()
                    for h in range(GH):
                        o.MM(ps[0:64, h * C:(h + 1) * C], X["sAt"][:, h, :], S0b[:, h, :], True, False)
                        o.MM(ps[0:64, h * C:(h + 1) * C], Aak[:, h, :], Vtok[:, h, :], False, True)
                    o.CP("act", WT, ps3(ps))
                    yield
                    ps = o.pst()
                    permm(ps, lambda h: Tf[:, h, :], lambda h: WT[:, h, :])
                    o.CP("act", UT, ps3(ps))
                    yield
                    ps = o.pst()
                    for h in range(GH):
                        sl = ps[0:64, h * C:(h + 1) * C]
                        o.MM(sl, S0b[:, h, :], X["sRt"][:, h, :], True, False)
                        o.MM(sl, UT[:, h, :], Arb[:, h, :], False, False)
                        o.MM(sl, Vtok[:, h, :], Ark[:, h, :], False, True)
                    o.CP("dve", Y, ps3(ps))
                    ps = o.pst()
                    for h in range(GH):
                        sl = ps[0:64, h * C:(h + 1) * C]
                        o.MM(sl, Bhtok[:, h, :], UT[:, h, :], True, False)
                        o.MM(sl, Khtok[:, h, :], Vtok[:, h, :], False, True)
                    pcb = PCt.v(PCt.ap[:, :, ci:ci + 1].to_broadcast([64, GH, C]))
                    o.T2("pool", S1, S0, pcb, ALU.mult)
                    o.T2("dve", S1, S1, ps3(ps), ALU.add)
                    o.CP("act", S1b, S1)
                    cur = 1 - cur
                    yield
                    o.A(Ysq, Y, AF.Square)
                    ps1 = o.pst()
                    o.MM(ps1[0:64, 0:W], ones64, flat(Y), True, True)
                    ps2 = o.pst()
                    o.MM(ps2[0:64, 0:W], ones64, flat(Ysq), True, True)
                    o.A(flat(mean), ps1[0:64, 0:W], AF.Copy, scale=1.0 / 64)
                    o.T2("pool", rstd, mean, mean, ALU.mult)
                    o.STT("dve", flat(rstd), ps2[0:64, 0:W], 1.0 / 64, flat(rstd), ALU.mult, ALU.subtract)
                    o.TS("dve", rstd, rstd, GN_EPS, None, ALU.add)
                    o.A(rstd, rstd, AF.Sqrt)
                    o.RCP(rstd, rstd)
                    o.T2("pool", yo, Y, mean, ALU.subtract)
                    o.T2("pool", yo, yo, rstd, ALU.mult)
                    gg = gng.v(gng.ap[:, h0:h0 + GH].unsqueeze(2).to_broadcast([64, GH, C]))
                    gb = gnb.v(gnb.ap[:, h0:h0 + GH].unsqueeze(2).to_broadcast([64, GH, C]))
                    o.T2("dve", yo, yo, gg, ALU.mult)
                    o.T2("dve", yo, yo, gb, ALU.add)
                    o.T2("dve", yo, yo, X["sbv"], ALU.add)
                    o.T2("pool", yo, yo, X["sg"], ALU.mult)
                    t0 = tok0 + cc * C
                    o.ST("sp", d["ybT"][rows, t0:t0 + C].rearrange("(h j) t -> j h t", j=64), yo)
                    yield

        NSLOT = min(2, NG)
        slots = [make_slot() for _ in range(NSLOT)]
        for g0 in range(0, NG, NSLOT):
            gens = [run_group(g0 + i, slots[i]) for i in range(NSLOT) if g0 + i < NG]
            live = list(gens)
            while live:
                for gn in list(live):
                    try:
                        next(gn)
                    except StopIteration:
                        live.remove(gn)
        cx.end_stage()


def stage_mixout(cx, l, xin, xout):
    cfg, d = cx.cfg, cx.dram
    o = Ops(cx)
    D, S, NB, TT, CB = cfg.D, cfg.S, cfg.NB, cfg.TT, cfg.CB
    CBB, KC = CB // 128, D // 128
    gB0 = cfg.c1 + D
    Wr = d["w_rwkv_proj"][l].rearrange("(kc p) n -> p kc n", p=128)
    Wo = d["w_out"][l].rearrange("(kc p) n -> p kc n", p=128)
    KM = max(CBB, KC)
    with ExitStack() as es:
        wb = [o.tile(es, [128, KM, 512], BF16, "wb") for _ in range(2)]
        yb = o.tile(es, [128, CBB, TT], BF16, "yb")
        mix = o.tile(es, [128, KC, TT], BF16, "mix")
        gat = [o.tile(es, [128, TT], F32, "gat") for _ in range(2)]
        ma = [o.tile(es, [128, TT], F32, "ma") for _ in range(2)]
        xt = [o.tile(es, [128, D], F32, "xt") for _ in range(TT // 128)]
        ot = [o.tile(es, [128, 512], F32, "ot") for _ in range(2)]
        bc = Bcast(cx, es)
        GT = Tl(bc.GT, bc.dt)
        nw = 0
        ntp = 0
        for b in range(NB):
            bc.build(cx, l, b, 1)
            for st in range(S // TT):
                tok0 = b * S + st * TT
                o.LD("pool", yb, d["ybT"][:, tok0:tok0 + TT].rearrange("(kc p) t -> p kc t", p=128))
                for c0 in range(0, D, 512):
                    cw = min(512, D - c0)
                    w = wb[nw % 2]
                    nw += 1
                    o.LD("pool", w[:, 0:CBB, 0:cw], Wr[:, :, c0:c0 + cw])
                    for s0 in range(0, cw, 128):
                        nb = (c0 + s0) // 128
                        s = nb % 2
                        o.LD("sp", gat[s], d["projT"][gB0 + nb * 128:gB0 + (nb + 1) * 128, tok0:tok0 + TT])
                        o.LD("sp", ma[s], d["mixAT"][nb * 128:(nb + 1) * 128, tok0:tok0 + TT])
                        o.A(gat[s], gat[s], AF.Sigmoid)
                        ps = o.pst()
                        for kc in range(CBB):
                            o.MM(ps[:, 0:TT], w[:, kc, s0:s0 + 128], yb[:, kc, :], kc == 0, kc == CBB - 1)
                        o.T2("dve", gat[s], ps[:, 0:TT], gat[s], ALU.mult)
                        o.T2("pool", mix[:, nb, :], gat[s], ma[s], ALU.add)
                NT = TT // 128
                for tt in range(NT):
                    r0 = tok0 + tt * 128
                    o.LD("sp", xt[tt], d[xin][r0:r0 + 128, :])
                for c0 in range(0, D, 512):
                    cw = min(512, D - c0)
                    w = wb[nw % 2]
                    nw += 1
                    o.LD("pool", w[:, 0:KC, 0:cw], Wo[:, :, c0:c0 + cw])
                    for tt in range(NT):
                        ps = o.pst()
                        for kc in range(KC):
                            o.MM(ps[:, 0:cw], mix[:, kc, tt * 128:(tt + 1) * 128], w[:, kc, 0:cw], kc == 0, kc == KC - 1)
                        tp = ot[ntp % 2]
                        ntp += 1
                        o.T2("dve", tp[:, 0:cw], ps[:, 0:cw], GT[:, c0:c0 + cw], ALU.mult)
                        o.T2("pool", xt[tt][:, c0:c0 + cw], xt[tt][:, c0:c0 + cw], tp[:, 0:cw], ALU.add)
                for tt in range(NT):
                    r0 = tok0 + tt * 128
                    o.ST("sp", d[xout][r0:r0 + 128, :], xt[tt])
        cx.end_stage()


I32 = mybir.dt.int32


def moe_dims(cfg):
    import os
    BLK = 512 if cfg.TOK >= 2048 else int(os.environ.get("MOE_BLK", "128"))
    NBLK = (4 * cfg.TOK) // BLK + cfg.E
    NJ = cfg.TOK // BLK + 1
    return BLK, NBLK, NJ


def moe_pieces(cfg):
    NPG = cfg.DE // 128
    DW = min(512, cfg.D)
    NPD = cfg.D // DW
    return NPG, DW, NPD


def _gather(o, out, dram2d, idx):
    oa, ia = out.ap, idx.ap
    return o.fw.dma("pool", lambda e: e.indirect_dma_start(
        out=oa, out_offset=None, in_=dram2d, in_offset=bass.IndirectOffsetOnAxis(ap=ia, axis=0)),
        reads=[idx.d], writes=[out.d])


def _scatter(o, dram2d, idx, in_):
    ia, sa = idx.ap, in_.ap
    return o.fw.dma("pool", lambda e: e.indirect_dma_start(
        out=dram2d, out_offset=bass.IndirectOffsetOnAxis(ap=ia, axis=0), in_=sa, in_offset=None),
        reads=[idx.d, in_.d])


def stage_route(cx, l, xin):
    cfg, d = cx.cfg, cx.dram
    o = Ops(cx)
    D, S, NB, E, TOK, DE = cfg.D, cfg.S, cfg.NB, cfg.E, cfg.TOK, cfg.DE
    KC, FC = D // 128, DE // 128
    NTL = TOK // 128
    BLK, NBLK, NJ = moe_dims(cfg)
    with ExitStack() as es:
        K = o.consts(es)
        lstrict = o.tile(es, [128, 128], F32, "lstrict")
        o.LD("sp", lstrict, d["c_lstrict"])
        rw = o.tile(es, [128, KC, E], F32, "rw")
        o.LD("sp", rw, d["router_w"][l].rearrange("(kc p) e -> p kc e", p=128))
        rb = o.tile(es, [1, E], F32, "rb")
        o.LD("sp", rb, d["router_b"][l])
        xt = [o.tile(es, [128, D], F32, "xt") for _ in range(2)]
        ut = Tl(cx.sb(es, [128, D], F32, "ut"))
        junk = o.tile(es, [128, D], F32, "junk")
        small = o.tile(es, [128, 4], F32, "small")
        hT32 = o.tile(es, [128, KC, 128], F32, "hT32")
        lg = o.tile(es, [128, NTL, E], F32, "lg")
        t8 = o.tile(es, [128, NTL, 8], F32, "t8")
        mask = o.tile(es, [128, E], F32, "mask")
        rank = o.tile(es, [128, NTL, E], F32, "rank")
        carry = o.tile(es, [128, E], F32, "carry")
        o.MS("dve", carry, 0.0)
        bc = Bcast(cx, es)
        i = 0
        for b in range(NB):
            bc.build(cx, l, b, 2)
            for tl in range(S // 128):
                s = i % 2
                r0 = i * 128
                o.LD("sp", xt[s], d[xin][r0:r0 + 128, :])
                norm_mod_tile(cx, xt[s].ap, xt[s].d, ut.ap, ut.d, junk.ap, junk.d, small.ap, small.d,
                              bc.GS, bc.dg, bc.SH, bc.ds, D)
                o.ST("sp", d["Hd"][r0:r0 + 128, :], ut)
                transpose_to_fm(cx, {"ident": K["ident"].ap, "dep": K["ident"].d}, ut.ap, ut.d, KC,
                                [(hT32.ap, hT32.d, "act")])
                ps = o.pst()
                for kc in range(KC):
                    o.MM(ps[:, 0:E], hT32[:, kc, :], rw[:, kc, :], kc == 0, False)
                o.MM(ps[:, 0:E], K["ones128"][0:1, :], rb, False, True)
                o.CP("dve", lg[:, i, :], ps[:, 0:E])
                t8a, lga = t8.ap[:, i, :], lg.ap[:, i, :]
                cx.fw.op("dve", lambda e, t8a=t8a, lga=lga: e.max(out=t8a, in_=lga), reads=[lg.d], writes=[t8.d])
                o.TS("dve", mask, lg[:, i, :], t8[:, i, 3:4], None, ALU.is_ge)
                ps2 = o.pst()
                o.MM(ps2[:, 0:E], lstrict, mask, True, True)
                ps3 = o.pst()
                o.MM(ps3[:, 0:E], K["ones128"], mask, True, True)
                o.T2("dve", rank[:, i, :], ps2[:, 0:E], carry, ALU.add)
                o.T2("dve", carry, carry, ps3[:, 0:E], ALU.add)
                i += 1
        thr = o.tile(es, [128, E, NJ], F32, "thr")
        o.LD("sp", thr, d["c_thr"])
        cmp3 = o.tile(es, [128, E, NJ], F32, "cmp3")
        o.T2("dve", cmp3, carry.v(carry.ap.unsqueeze(2).to_broadcast([128, E, NJ])), thr, ALU.is_gt)
        nblk = o.tile(es, [128, E], F32, "nblk")
        ca, na = cmp3.ap, nblk.ap
        cx.fw.op("dve", lambda e: e.reduce_sum(out=na, in_=ca, axis=AX.X), reads=[cmp3.d], writes=[nblk.d])
        onesE = o.tile(es, [128, E], F32, "onesE")
        o.MS("dve", onesE, 1.0)
        incl = o.tile(es, [128, E], F32, "incl")
        ia_, oa_ = incl.ap, onesE.ap
        cx.fw.op("dve", lambda e: e.tensor_tensor_scan(out=ia_, data0=oa_, data1=na, initial=0.0, op0=ALU.mult, op1=ALU.add),
                 reads=[nblk.d, onesE.d], writes=[incl.d])
        pstart = o.tile(es, [128, E], F32, "pstart")
        pend = o.tile(es, [128, E], F32, "pend")
        o.T2("dve", pstart, incl, nblk, ALU.subtract)
        o.TS("dve", pstart, pstart, float(BLK), None, ALU.mult)
        o.TS("dve", pend, incl, float(BLK), None, ALU.mult)
        bst = o.tile(es, [128, NBLK, E], F32, "bst")
        o.LD("sp", bst, d["c_blkstart"])
        o.T2("dve", bst, pend.v(pend.ap.unsqueeze(1).to_broadcast([128, NBLK, E])), bst, ALU.is_le)
        be = o.tile(es, [128, NBLK], F32, "be")
        ba, bsa = be.ap, bst.ap
        cx.fw.op("dve", lambda e: e.reduce_sum(out=ba, in_=bsa, axis=AX.X), reads=[bst.d], writes=[be.d])
        o.TS("dve", be, be, float(E - 1), None, ALU.min)
        NPG, DW, NPD = moe_pieces(cfg)
        for nm, n2, mul, offc in (("idx_gu", NPG, 128.0 * NPG, "c_guoff"), ("idx_d", NPD, 128.0 * NPD, "c_doff"),
                                  ("idx_b", 2, None, "c_boff")):
            off = o.tile(es, [128, n2], F32, "off_" + nm)
            o.LD("sp", off, d[offc])
            tf = o.tile(es, [128, NBLK, n2], F32, "tf_" + nm)
            ti = o.tile(es, [128, NBLK, n2], I32, "ti_" + nm)
            beb = be.v(be.ap.unsqueeze(2).to_broadcast([128, NBLK, n2]))
            offb = off.v(off.ap.unsqueeze(1).to_broadcast([128, NBLK, n2]))
            if mul is not None:
                o.STT("dve", tf, beb, mul, offb, ALU.mult, ALU.add)
            else:
                mcol = o.tile(es, [128, 2], F32, "mcol")
                o.LD("sp", mcol, d["c_bmul"])
                o.T2("dve", tf, beb, mcol.v(mcol.ap.unsqueeze(1).to_broadcast([128, NBLK, n2])), ALU.mult)
                o.T2("dve", tf, tf, offb, ALU.add)
            o.CP("dve", ti, tf)
            o.ST("sp", d[nm], ti)
        w4 = o.tile(es, [128, NTL, 4], F32, "w4")
        o.T2("dve", w4, t8[:, :, 0:4], t8.v(t8.ap[:, :, 0:1].to_broadcast([128, NTL, 4])), ALU.subtract)
        o.A(w4, w4, AF.Exp)
        ssum = o.tile(es, [128, NTL], F32, "ssum")
        wa, sa = w4.ap, ssum.ap
        cx.fw.op("dve", lambda e: e.reduce_sum(out=sa, in_=wa, axis=AX.X), reads=[w4.d], writes=[ssum.d])
        o.RCP(ssum, ssum)
        o.T2("dve", w4, w4, ssum.v(ssum.ap.unsqueeze(2).to_broadcast([128, NTL, 4])), ALU.mult)
        o.ST("sp", d["w4"], w4)
        o.T2("dve", rank, rank, pstart.v(pstart.ap.unsqueeze(1).to_broadcast([128, NTL, E])), ALU.add)
        sel = o.tile(es, [128, NTL, E], F32, "sel")
        d4f = o.tile(es, [128, NTL, 4], F32, "d4f")
        d4 = o.tile(es, [128, NTL, 4], I32, "d4")
        for k in range(4):
            o.T2("dve", sel, lg, t8.v(t8.ap[:, :, k:k + 1].to_broadcast([128, NTL, E])), ALU.is_equal)
            o.T2("dve", sel, sel, rank, ALU.mult)
            sla, dka = sel.ap, d4f.ap[:, :, k]
            cx.fw.op("dve", lambda e, sla=sla, dka=dka: e.reduce_sum(out=dka, in_=sla, axis=AX.X),
                     reads=[sel.d], writes=[d4f.d])
        o.CP("dve", d4, d4f)
        o.ST("sp", d["d4"], d4)
        for i in range(NTL):
            s = i % 2
            o.LD("sp", xt[s], d["Hd"][i * 128:(i + 1) * 128, :])
            for k in range(4):
                _scatter(o, d["Hsorted"], d4[:, i, k:k + 1], xt[s])
        cx.end_stage()


def stage_experts(cx, l):
    cfg, d = cx.cfg, cx.dram
    o = Ops(cx)
    D, E, DE = cfg.D, cfg.E, cfg.DE
    KC, FC = D // 128, DE // 128
    BLK, NBLK, NJ = moe_dims(cfg)
    NT = BLK // 128
    NPG, DW, NPD = moe_pieces(cfg)
    wgu, wd = d["w_gu3_%d" % l], d["w_d3_%d" % l]
    bgu2, bd2 = d["b_gu2_%d" % l], d["b_d2_%d" % l]
    GW = KC * 256
    DWW = FC * DW
    SW = max(GW, DWW)
    with ExitStack() as es:
        K = o.consts(es)
        igu = o.tile(es, [128, NBLK, NPG], I32, "igu")
        o.LD("sp", igu, d["idx_gu"])
        idd = o.tile(es, [128, NBLK, NPD], I32, "idd")
        o.LD("sp", idd, d["idx_d"])
        ib = o.tile(es, [128, NBLK, 2], I32, "ib")
        o.LD("sp", ib, d["idx_b"])
        stgw = [o.tile(es, [128, SW], F32, "stgw") for _ in range(2)]
        wg = [o.tile(es, [128, KC, 256], BF16, "wg") for _ in range(3)]
        wdn = [o.tile(es, [128, FC, DW], BF16, "wdn") for _ in range(3)]
        bg = [o.tile(es, [128, 2 * FC], F32, "bg") for _ in range(2)]
        bdb = [o.tile(es, [128, D], F32, "bdb") for _ in range(2)]
        ht = [Tl(cx.sb(es, [128, D], F32, "ht")) for _ in range(NT)]
        hTb = o.tile(es, [128, KC, BLK], BF16, "hTb")
        actg = o.tile(es, [128, FC, BLK], BF16, "actg")
        gc = [o.tile(es, [128, BLK], F32, "gc") for _ in range(2)]
        sg = [o.tile(es, [128, BLK], F32, "sg") for _ in range(2)]
        uc = [o.tile(es, [128, BLK], F32, "uc") for _ in range(2)]
        yst = [o.tile(es, [128, DW], F32, "yst") for _ in range(4)]
        cnt = {"stg": 0, "wg": 0, "wd": 0, "act": 0, "yst": 0, "cast": 0}

        def fetch(kind, blk, piece):
            st = stgw[cnt["stg"] % 2]
            cnt["stg"] += 1
            if kind == "g":
                dst = wg[cnt["wg"] % 3]
                cnt["wg"] += 1
                _gather(o, st[:, 0:GW], wgu, igu[:, blk, piece:piece + 1])
                src = st.v(st.ap[:, 0:GW].rearrange("p (k n) -> p k n", k=KC))
            else:
                dst = wdn[cnt["wd"] % 3]
                cnt["wd"] += 1
                _gather(o, st[:, 0:DWW], wd, idd[:, blk, piece:piece + 1])
                src = st.v(st.ap[:, 0:DWW].rearrange("p (k n) -> p k n", k=FC))
            nk = KC if kind == "g" else FC
            h = nk // 2
            o.CP("act", dst[:, 0:h, :], src[:, 0:h, :])
            o.CP("dve", dst[:, h:nk, :], src[:, h:nk, :])
            return dst

        order = [(blk, kind, pc) for blk in range(NBLK) for kind, n in (("g", NPG), ("d", NPD)) for pc in range(n)]
        fetched = {}

        def ensure(k):
            if k < len(order) and k not in fetched:
                fetched[k] = fetch(order[k][1], order[k][0], order[k][2])

        ensure(0)
        k = 0
        for tt in range(NT):
            o.LD("sp", ht[tt], d["Hsorted"][tt * 128:(tt + 1) * 128, :])
        for blk in range(NBLK):
            s = blk % 2
            _gather(o, bg[s], bgu2, ib[:, blk, 0:1])
            _gather(o, bdb[s], bd2, ib[:, blk, 1:2])
            for tt in range(NT):
                transpose_to_fm(cx, {"ident": K["ident"].ap, "dep": K["ident"].d}, ht[tt].ap, ht[tt].d, KC,
                                [(hTb.ap[:, :, tt * 128:(tt + 1) * 128], hTb.d, "alt")])
                if blk + 1 < NBLK:
                    r1 = (blk + 1) * BLK + tt * 128
                    o.LD("sp", ht[tt], d["Hsorted"][r1:r1 + 128, :])
            for fidx in range(NPG):
                w = fetched.pop(k)
                ensure(k + 1)
                k += 1
                psg = o.pst()
                for kc in range(KC):
                    o.MM(psg[:, 0:BLK], w[:, kc, 0:128], hTb[:, kc, :], kc == 0, kc == KC - 1)
                psu = o.pst()
                for kc in range(KC):
                    o.MM(psu[:, 0:BLK], w[:, kc, 128:256], hTb[:, kc, :], kc == 0, kc == KC - 1)
                a = cnt["act"] % 2
                cnt["act"] += 1
                o.TS("dve", gc[a], psg[:, 0:BLK], bg[s][:, fidx:fidx + 1], SW_LIMIT, ALU.add, ALU.min)
                o.A(sg[a], gc[a], AF.Sigmoid, scale=SW_ALPHA)
                o.TS("dve", uc[a], psu[:, 0:BLK], bg[s][:, FC + fidx:FC + fidx + 1], SW_LIMIT, ALU.add, ALU.min)
                o.TS("dve", uc[a], uc[a], -SW_LIMIT, 1.0, ALU.max, ALU.add)
                o.T2("dve", gc[a], gc[a], sg[a], ALU.mult)
                o.T2("dve", actg[:, fidx, :], gc[a], uc[a], ALU.mult)
            for pc in range(NPD):
                w = fetched.pop(k)
                ensure(k + 1)
                k += 1
                for tt in range(NT):
                    ps = o.pst()
                    for fc in range(FC):
                        o.MM(ps[:, 0:DW], actg[:, fc, tt * 128:(tt + 1) * 128], w[:, fc, :], fc == 0, fc == FC - 1)
                    col = pc * DW
                    y_ = yst[cnt["yst"] % 4]
                    cnt["yst"] += 1
                    o.T2("dve", y_, ps[:, 0:DW], bdb[s][:, col:col + DW], ALU.add)
                    r0 = blk * BLK + tt * 128
                    o.ST("sp", d["Ybuf"][r0:r0 + 128, col:col + DW], y_)
        cx.end_stage()


def stage_combine(cx, l, xin, xout, final=False):
    cfg, d = cx.cfg, cx.dram
    o = Ops(cx)
    D, S, NB, TOK = cfg.D, cfg.S, cfg.NB, cfg.TOK
    NTL = TOK // 128
    with ExitStack() as es:
        d4 = o.tile(es, [128, NTL, 4], I32, "d4")
        o.LD("sp", d4, d["d4"])
        w4 = o.tile(es, [128, NTL, 4], F32, "w4")
        o.LD("sp", w4, d["w4"])
        yk = [o.tile(es, [128, D], F32, "yk") for _ in range(4)]
        xt = [o.tile(es, [128, D], F32, "xt") for _ in range(2)]
        acc = [o.tile(es, [128, D], F32, "acc") for _ in range(2)]
        bc = Bcast(cx, es)
        GT = Tl(bc.GT, bc.dt)
        if final:
            fg = o.tile(es, [128, D], F32, "fg")
            o.LD("sp", fg, d["final_g"][0:1, :].partition_broadcast(128))
            junk = o.tile(es, [128, D], F32, "junk")
            small = o.tile(es, [128, 4], F32, "small")
        i = 0
        for b in range(NB):
            bc.build(cx, l, b, 2)
            for tl in range(S // 128):
                s = i % 2
                r0 = i * 128
                o.LD("sp", xt[s], d[xin][r0:r0 + 128, :])
                for k in range(4):
                    _gather(o, yk[k], d["Ybuf"], d4[:, i, k:k + 1])
                a = acc[s]
                o.TS("dve", a, yk[0], w4[:, i, 0:1], None, ALU.mult)
                for k in range(1, 4):
                    o.STT("dve", a, yk[k], w4[:, i, k:k + 1], a, ALU.mult, ALU.add)
                o.T2("dve", a, a, GT, ALU.mult)
                o.T2("dve", a, a, xt[s], ALU.add)
                if final:
                    norm_mod_tile(cx, a.ap, a.d, xt[s].ap, xt[s].d, junk.ap, junk.d, small.ap, small.d,
                                  fg.ap, fg.d, None, None, D)
                    o.ST("sp", d["out"][r0:r0 + 128, :], xt[s])
                else:
                    o.ST("sp", d[xout][r0:r0 + 128, :], a)
                i += 1
        cx.end_stage()


def all_stages(cfg):
    st = []
    for l in range(cfg.L):
        st.append(lambda cx, l=l: stage_mod(cx, l))
    xin = "x"
    for l in range(cfg.L):
        xm = "x%d" % (2 * l + 1)
        xo = "x%d" % (2 * l + 2)
        last = l == cfg.L - 1
        st.append(lambda cx, l=l, xin=xin: stage_inproj(cx, l, xin))
        st.append(lambda cx, l=l: stage_conv(cx, l))
        st.append(lambda cx, l=l: stage_rwkv_prep(cx, l))
        st.append(lambda cx, l=l: stage_rwkv_scan(cx, l))
        st.append(lambda cx, l=l, xin=xin, xm=xm: stage_mixout(cx, l, xin, xm))
        st.append(lambda cx, l=l, xm=xm: stage_route(cx, l, xm))
        st.append(lambda cx, l=l: stage_experts(cx, l))
        st.append(lambda cx, l=l, xm=xm, xo=xo, last=last: stage_combine(cx, l, xm, xo, final=last))
        xin = xo
    return st


def shared_prep(cfg, inp):
    m = host_prep(cfg, inp, 0)
    m.pop("x")
    m.pop("cT")
    return m


def core_prep(cfg, inp, core):
    c = cfg
    KC = c.D // 128
    b0 = core * c.NB
    x = np.ascontiguousarray(inp["x"][b0:b0 + c.NB].reshape(c.NB * c.S, c.D), dtype=np.float32)
    cT = np.ascontiguousarray(inp["c"][b0:b0 + c.NB].T.reshape(KC, 128, c.NB).transpose(1, 0, 2), dtype=np.float32)
    return {"x": x, "cT": cT}


def kernel(**inputs):
    cfg = Cfg()
    inp = {k: np.asarray(v) for k, v in inputs.items()}
    nc = build_program(cfg, all_stages(cfg), outputs=("out",))
    shared = shared_prep(cfg, inp)
    in_maps = []
    for core in range(cfg.NCORES):
        m = dict(shared)
        m.update(core_prep(cfg, inp, core))
        in_maps.append(m)
    res = run_bass_kernel_spmd(nc, in_maps, core_ids=list(range(cfg.NCORES)))
    B = cfg.NB * cfg.NCORES
    out = np.concatenate([np.asarray(r["out"], dtype=np.float32).reshape(cfg.NB, cfg.S, cfg.D) for r in res.results], axis=0)
    return out.reshape(B, cfg.S, cfg.D)
```
